# Optimizing a Trainium2 kernel written in Bass

```python
import jax, jax.numpy as jnp
from jax import lax
import numpy as np

D_MODEL = 1024
BATCH = 16
SEQ = 2048
DEPTH = 4

CHUNK = 64
HEAD_DIM = 64
FOX_HEADS = 8
HGRN_HEADS = 8
FOX_WIDTH = FOX_HEADS * HEAD_DIM
HGRN_WIDTH = HGRN_HEADS * HEAD_DIM
N_BRANCHES = 2
Q_BLOCK = 128
N_EXPERTS = 16
N_GROUPS = 4
EXPERTS_PER_GROUP = N_EXPERTS // N_GROUPS
TOP_K = 2
D_EXPERT = D_MODEL
MOE_BLOCK = 512
LN_EPS = 1e-5
RMS_EPS = 1e-6
MASK_VALUE = -1e30
ALPHA = (2 * DEPTH) ** 0.25
BETA = (8 * DEPTH) ** -0.25
IN_SIZES = (FOX_WIDTH, FOX_WIDTH, FOX_WIDTH, FOX_HEADS, HGRN_WIDTH, HGRN_WIDTH, HGRN_WIDTH, HGRN_WIDTH, N_BRANCHES * D_MODEL)
N_IN = sum(IN_SIZES)
VALUE_SLOTS = (2, 6)
FOX_F_SLOT = 3

kernel_name = 'hybrid_fox_hgrn2_grouped_moe_deepnorm'


def layer_norm(x, g, b):
    xf = x.astype(jnp.float32)
    mu = xf.mean(-1, keepdims=True)
    var = jnp.square(xf - mu).mean(-1, keepdims=True)
    return ((xf - mu) * lax.rsqrt(var + LN_EPS) * g + b).astype(x.dtype)


def split_cols(h, sizes):
    out, off = [], 0
    for s in sizes:
        out.append(h[..., off:off + s])
        off += s
    return out


def to_heads(t, n_heads):
    B, S, W = t.shape
    return t.reshape(B, S, n_heads, W // n_heads).transpose(0, 2, 1, 3)


def from_heads(t):
    B, H, S, d = t.shape
    return t.transpose(0, 2, 1, 3).reshape(B, S, H * d)


def fox_attention(q, k, v, logf):
    B, H, S, d = q.shape
    F = jnp.cumsum(logf, axis=-1)
    scale = d ** -0.5
    outs = []
    for blk in range(S // Q_BLOCK):
        q0, q1 = blk * Q_BLOCK, (blk + 1) * Q_BLOCK
        s = jnp.einsum('bhqd,bhkd->bhqk', q[:, :, q0:q1], k[:, :, :q1]).astype(jnp.float32) * scale
        s = s + F[:, :, q0:q1, None] - F[:, :, None, :q1]
        qpos = jnp.arange(q0, q1)[:, None]
        kpos = jnp.arange(q1)[None, :]
        s = jnp.where(kpos <= qpos, s, MASK_VALUE)
        p = jax.nn.softmax(s, axis=-1)
        outs.append(jnp.einsum('bhqk,bhkd->bhqd', p.astype(v.dtype), v[:, :, :q1]))
    return jnp.concatenate(outs, axis=2)


def hgrn2_chunkwise(q, k, v, logf):
    B, H, S, dk = q.shape
    dv = v.shape[-1]
    n = S // CHUNK

    def to_chunks(t):
        return jnp.moveaxis(t.reshape(B, H, n, CHUNK, t.shape[-1]), 2, 0)

    causal = jnp.tril(jnp.ones((CHUNK, CHUNK), dtype=bool))[:, :, None]

    def step(state, inp):
        qt, kt, vt, gt = inp
        b = jnp.cumsum(gt, axis=-2)
        b_last = b[:, :, -1:, :]
        inter = jnp.einsum('bhtd,bhde->bhte', qt * jnp.exp(b), state)
        diff = b[:, :, :, None, :] - b[:, :, None, :, :]
        decay = jnp.where(causal, jnp.exp(jnp.where(causal, diff, 0.0)), 0.0)
        scores = jnp.einsum('bhtsd,bhsd->bhts', qt[:, :, :, None, :] * decay, kt)
        intra = jnp.einsum('bhts,bhse->bhte', scores, vt)
        state = jnp.exp(b_last[:, :, 0, :, None]) * state + jnp.einsum('bhsd,bhse->bhde', kt * jnp.exp(b_last - b), vt)
        return state, inter + intra

    s0 = jnp.zeros((B, H, dk, dv), jnp.float32)
    _, o = lax.scan(step, s0, (to_chunks(q), to_chunks(k), to_chunks(v), to_chunks(logf)))
    return jnp.moveaxis(o, 0, 2).reshape(B, H, S, dv)


def grouped_top2_moe(x, router_w, router_b, w1, w3, w2):
    B, S, D = x.shape
    T = B * S
    xt = x.reshape(T, D)
    logits = (xt @ router_w).astype(jnp.float32) + router_b.astype(jnp.float32)
    probs = jax.nn.softmax(logits, axis=-1)
    grouped = probs.reshape(T, N_GROUPS, EXPERTS_PER_GROUP)
    g_sel = jnp.argmax(grouped.max(-1), axis=-1)
    in_group = jnp.take_along_axis(grouped, g_sel[:, None, None], axis=1)[:, 0]
    top_p, top_local = lax.top_k(in_group, TOP_K)
    expert_idx = g_sel[:, None] * EXPERTS_PER_GROUP + top_local
    gate = top_p / top_p.sum(-1, keepdims=True)

    A = T * TOP_K
    flat_e = expert_idx.reshape(A)
    flat_tok = jnp.repeat(jnp.arange(T, dtype=jnp.int32), TOP_K)
    flat_gate = gate.reshape(A)
    order = jnp.argsort(flat_e)
    sorted_e = flat_e[order]
    counts = jnp.bincount(flat_e, length=N_EXPERTS)
    padded = (counts + MOE_BLOCK - 1) // MOE_BLOCK * MOE_BLOCK
    start = jnp.cumsum(counts) - counts
    pend = jnp.cumsum(padded)
    pstart = pend - padded
    dest = pstart[sorted_e] + (jnp.arange(A) - start[sorted_e])
    n_blocks = (A + N_EXPERTS * (MOE_BLOCK - 1) + MOE_BLOCK - 1) // MOE_BLOCK
    P = n_blocks * MOE_BLOCK
    row_tok = jnp.full((P,), T, jnp.int32).at[dest].set(flat_tok[order])
    row_gate = jnp.zeros((P,), jnp.float32).at[dest].set(flat_gate[order])
    block_e = jnp.minimum(jnp.searchsorted(pend, jnp.arange(n_blocks) * MOE_BLOCK, side='right'), N_EXPERTS - 1)
    x_pad = jnp.concatenate([xt, jnp.zeros((1, D), xt.dtype)], axis=0)
    xb = x_pad[row_tok].reshape(n_blocks, MOE_BLOCK, D)

    def expert_rows(args):
        xr, e = args
        h = jax.nn.silu(xr @ w1[e]) * (xr @ w3[e])
        return h @ w2[e]

    yb = lax.map(expert_rows, (xb, block_e)).reshape(P, D)
    y = jax.ops.segment_sum(yb * row_gate[:, None].astype(yb.dtype), row_tok, num_segments=T + 1)[:T]
    return y.reshape(B, S, D).astype(x.dtype)


def setup_inputs(seed: int = 0) -> dict:
    key = jax.random.key(seed)
    ks = jax.random.split(key, 24)
    nrm = jax.random.normal
    f32 = jnp.float32
    col_scale = jnp.concatenate([jnp.full((s,), BETA if i in VALUE_SLOTS else 1.0, f32) for i, s in enumerate(IN_SIZES)])
    fox_f_off = sum(IN_SIZES[:FOX_F_SLOT])
    b_in = 0.02 * nrm(ks[4], (DEPTH, N_IN), f32)
    b_in = b_in.at[:, fox_f_off:fox_f_off + FOX_HEADS].add(jnp.linspace(1.0, 4.0, FOX_HEADS))
    return {
        'x': nrm(ks[0], (BATCH, SEQ, D_MODEL), f32),
        'ln_in_g': 1.0 + 0.02 * nrm(ks[1], (D_MODEL,), f32),
        'ln_in_b': 0.02 * nrm(ks[2], (D_MODEL,), f32),
        'w_in': nrm(ks[3], (DEPTH, D_MODEL, N_IN), f32) * D_MODEL ** -0.5 * col_scale,
        'b_in': b_in,
        'w_fox_branch': nrm(ks[5], (DEPTH, FOX_WIDTH, D_MODEL), f32) * FOX_WIDTH ** -0.5 * BETA,
        'hgrn_lb_logits': 0.1 * nrm(ks[6], (DEPTH, HGRN_WIDTH), f32),
        'hgrn_norm_g': 1.0 + 0.02 * nrm(ks[7], (DEPTH, HGRN_WIDTH), f32),
        'w_hgrn_branch': nrm(ks[8], (DEPTH, HGRN_WIDTH, D_MODEL), f32) * HGRN_WIDTH ** -0.5 * BETA,
        'w_mix_out': nrm(ks[9], (DEPTH, D_MODEL, D_MODEL), f32) * D_MODEL ** -0.5 * BETA,
        'b_mix_out': 0.02 * nrm(ks[10], (DEPTH, D_MODEL), f32),
        'ln1_g': 1.0 + 0.02 * nrm(ks[11], (DEPTH, D_MODEL), f32),
        'ln1_b': 0.02 * nrm(ks[12], (DEPTH, D_MODEL), f32),
        'router_w': nrm(ks[13], (D_MODEL, N_EXPERTS), f32) * D_MODEL ** -0.5,
        'router_b': 0.01 * nrm(ks[14], (N_EXPERTS,), f32),
        'expert_w1': nrm(ks[15], (DEPTH, N_EXPERTS, D_MODEL, D_EXPERT), f32) * D_MODEL ** -0.5,
        'expert_w3': nrm(ks[16], (DEPTH, N_EXPERTS, D_MODEL, D_EXPERT), f32) * D_MODEL ** -0.5 * BETA,
        'expert_w2': nrm(ks[17], (DEPTH, N_EXPERTS, D_EXPERT, D_MODEL), f32) * D_EXPERT ** -0.5 * BETA,
        'ln2_g': 1.0 + 0.02 * nrm(ks[18], (DEPTH, D_MODEL), f32),
        'ln2_b': 0.02 * nrm(ks[19], (DEPTH, D_MODEL), f32),
    }


def reference(x, ln_in_g, ln_in_b, w_in, b_in, w_fox_branch, hgrn_lb_logits, hgrn_norm_g, w_hgrn_branch,
              w_mix_out, b_mix_out, ln1_g, ln1_b, router_w, router_b, expert_w1, expert_w3, expert_w2,
              ln2_g, ln2_b):
    f32 = jnp.float32
    lb_p = jax.nn.softmax(hgrn_lb_logits.astype(f32), axis=0)
    lb_all = jnp.cumsum(lb_p, axis=0) - lb_p[0]
    x = layer_norm(x, ln_in_g, ln_in_b)
    for l in range(DEPTH):
        h = x @ w_in[l] + b_in[l]
        fq, fk, fv, ff, hq, hf, hi, hg, gates = split_cols(h, IN_SIZES)
        fox_logf = jax.nn.log_sigmoid(ff.astype(f32)).transpose(0, 2, 1)
        fox_o = fox_attention(to_heads(fq, FOX_HEADS), to_heads(fk, FOX_HEADS), to_heads(fv, FOX_HEADS), fox_logf)
        y_fox = from_heads(fox_o) @ w_fox_branch[l]
        lb = lb_all[l].reshape(HGRN_HEADS, 1, HEAD_DIM)
        z = to_heads(hf, HGRN_HEADS).astype(f32)
        f_gate = lb + (1.0 - lb) * jax.nn.sigmoid(z)
        logf = jnp.log(f_gate)
        k_in = (1.0 - lb) * jax.nn.sigmoid(-z)
        q_h = jax.nn.silu(to_heads(hq, HGRN_HEADS).astype(f32))
        o = hgrn2_chunkwise(q_h, k_in, to_heads(hi, HGRN_HEADS).astype(f32), logf)
        o = o * lax.rsqrt(jnp.mean(jnp.square(o), -1, keepdims=True) + RMS_EPS) * hgrn_norm_g[l].astype(f32).reshape(HGRN_HEADS, 1, HEAD_DIM)
        o = (from_heads(o) * jax.nn.silu(hg.astype(f32))).astype(x.dtype)
        y_hgrn = o @ w_hgrn_branch[l]
        g = jax.nn.sigmoid(gates)
        mixed = (g[..., :D_MODEL] * y_fox + g[..., D_MODEL:] * y_hgrn) @ w_mix_out[l] + b_mix_out[l]
        x = layer_norm(ALPHA * x + mixed, ln1_g[l], ln1_b[l])
        y_moe = grouped_top2_moe(x, router_w, router_b, expert_w1[l], expert_w3[l], expert_w2[l])
        x = layer_norm(ALPHA * x + y_moe, ln2_g[l], ln2_b[l])
    return x
```

```python
import numpy as np
from contextlib import ExitStack
import concourse.bass as bass
import concourse.mybir as mybir
from concourse.bass_utils import run_bass_kernel_spmd

F32 = mybir.dt.float32
BF16 = mybir.dt.bfloat16
AF = mybir.ActivationFunctionType
ALU = mybir.AluOpType
AX = mybir.AxisListType

D = 1024
NIN = 5640
NE = 16
ALPHA = float(8 ** 0.25)
LN_EPS = 1e-5
RMS_EPS = 1e-6
BIG = 1.0e4
ENGS = ('pe', 'act', 'dve', 'pool', 'sp')
NF = 76
NR = 8 + 2048 + 512
NCST = 902


class Prog:
    def __init__(self):
        self.q = {e: [] for e in ENGS}
        self.lw = {}
        self.rd = {}
        self.dma_cnt = {}
        self.fence_op = None

    def add(self, eng, fn, rd=(), wr=(), dma=None, nofence=False):
        idx = len(self.q[eng])
        me = (eng, idx)
        deps = set()
        for k in rd:
            w = self.lw.get(k)
            if w is not None:
                deps.add(w)
        for k in wr:
            w = self.lw.get(k)
            if w is not None:
                deps.add(w)
            for r in self.rd.get(k, ()):
                deps.add(r)
        if self.fence_op is not None and not nofence:
            deps.add(self.fence_op)
        deps.discard(me)
        for k in rd:
            self.rd.setdefault(k, []).append(me)
        for k in wr:
            self.lw[k] = me
            self.rd[k] = []
        rec = dict(fn=fn, deps=deps, dma=dma, inc=False, val=None)
        if dma is not None:
            self.dma_cnt[dma] = self.dma_cnt.get(dma, 0) + 16
            rec['val'] = self.dma_cnt[dma]
        self.q[eng].append(rec)
        return me

    def fence(self):
        deps = set()
        for k, w in self.lw.items():
            deps.add(w)
        for k, rs in self.rd.items():
            for r in rs:
                deps.add(r)
        if self.fence_op is not None:
            deps.add(self.fence_op)
        idx = len(self.q['dve'])
        me = ('dve', idx)
        deps.discard(me)
        self.q['dve'].append(dict(fn=lambda e: e.nop(), deps=deps, dma=None, inc=True, val=None))
        self.fence_op = me
        keep = lambda k: isinstance(k, tuple) and k[0] == 'ring'
        self.lw = {k: v for k, v in self.lw.items() if keep(k)}
        self.rd = {k: v for k, v in self.rd.items() if keep(k)}

    def finalize(self):
        q = self.q
        for e in ENGS:
            for op in q[e]:
                for (e2, i2) in op['deps']:
                    d = q[e2][i2]
                    if d['dma'] is None:
                        if e2 == 'pe' and e == 'pe' and op['dma'] is None:
                            continue
                        d['inc'] = True
        for e in ENGS:
            c = 0
            for op in q[e]:
                if op['dma'] is None and op['inc']:
                    c += 1
                    op['val'] = c
        for e in ENGS:
            waited = {}
            for op in q[e]:
                w = {}
                for (e2, i2) in op['deps']:
                    d = q[e2][i2]
                    if d['dma'] is None:
                        if e2 == 'pe' and e == 'pe' and op['dma'] is None:
                            continue
                        sk = ('eng', e2)
                    else:
                        sk = ('dma', d['dma'])
                    v = d['val']
                    if v > waited.get(sk, 0) and v > w.get(sk, 0):
                        w[sk] = v
                for sk, v in w.items():
                    waited[sk] = v
                op['waits'] = sorted(w.items(), key=str)

    def body(self, e, engsem, dmasem):
        def f(eng):
            for op in self.q[e]:
                for (sk, v) in op['waits']:
                    s = engsem[sk[1]] if sk[0] == 'eng' else dmasem[sk[1]]
                    eng.wait_ge(s, v)
                ins = op['fn'](eng)
                if op['dma'] is not None:
                    ins.then_inc(dmasem[op['dma']], 16)
                elif op['inc']:
                    ins.then_inc(engsem[e], 1)
            if e == 'sp':
                for k, v in self.dma_cnt.items():
                    eng.wait_ge(dmasem[k], v)
        return f


def make_consts():
    c = np.zeros((128, NCST), np.float32)
    i = np.arange(128)
    c[:, 0:128] = np.eye(128)
    c[:, 128:256] = (i[:, None] <= i[None, :])
    c[:, 256:384] = 1.0
    c[63, 384:512] = 1.0
    same = (i[:, None] // 64) == (i[None, :] // 64)
    tri = (i[:, None] <= i[None, :])
    mid = (i[:, None] % 64) <= 31
    c[:, 512:640] = same * (tri.astype(np.float32) - (mid & same).astype(np.float32))
    c[:, 640:768] = same * (1.0 - tri.astype(np.float32))
    c[:, 768] = (i // 64 == 0)
    c[:, 769] = (i // 64 == 1)
    c[:, 770] = (i // 64 == 0) & (i % 64 <= 31)
    c[:, 771] = (i // 64 == 1) & (i % 64 <= 31)
    c[:, 772:900] = same & tri
    c[:, 900] = 1.0
    c[:, 901] = (i % 64 <= 31)
    return c


def build(nseq, S, layers, dbg=None):
    NT = S // 128
    NTT = S // 512
    nc = bass.Bass("TRN2", target_bir_lowering=False)
    dt = nc.dram_tensor
    x_d = dt("x", [nseq, S, D], F32, kind="ExternalInput").ap()
    out_d = dt("out", [nseq, S, D], F32, kind="ExternalOutput").ap()
    win_d = dt("w_in", [4, D, NIN], F32, kind="ExternalInput").ap()
    wfb_d = dt("w_fb", [4, 512, D], F32, kind="ExternalInput").ap()
    whb_d = dt("w_hb", [4, 512, D], F32, kind="ExternalInput").ap()
    wmx_d = dt("w_mix", [4, D, D], F32, kind="ExternalInput").ap()
    w1_d = dt("w1", [4, NE, D, D], F32, kind="ExternalInput").ap()
    w3_d = dt("w3", [4, NE, D, D], F32, kind="ExternalInput").ap()
    w2_d = dt("w2", [4, NE, D, D], F32, kind="ExternalInput").ap()
    pf_d = dt("pf", [4, 128, NF], F32, kind="ExternalInput").ap()
    pfin_d = dt("pfin", [128, 16], F32, kind="ExternalInput").ap()
    pr_d = dt("pr", [4, 1, NR], F32, kind="ExternalInput").ap()
    lbl_d = dt("lbl", [1, 2048], F32, kind="ExternalInput").ap()
    rw_d = dt("rw", [128, 8 * 16], F32, kind="ExternalInput").ap()
    rb_d = dt("rb", [1, 16], F32, kind="ExternalInput").ap()
    cst_d = dt("cst", [128, NCST], F32, kind="ExternalInput").ap()
    dbg_d = None
    if dbg:
        dbg_d = dt("dbg", [len(dbg), D, S], F32, kind="ExternalOutput").ap()

    P = Prog()
    es = ExitStack()
    with es:
        SZ_X = 16 * S
        SZA = max(SZ_X, 32768)
        OFF_XH, OFF_XL, OFF_ACC = 0, SZ_X, SZ_X + SZA
        OFF_RING = OFF_ACC + 2 * SZA
        OFF_SM = OFF_RING + 4 * 8192
        TOTAL = OFF_SM + 16384
        arena = es.enter_context(nc.sbuf_tensor("arena", [128, TOTAL // 4], F32))

        def A(off, shape, dtp):
            assert off % 4 == 0
            nb = int(np.prod(shape[1:])) * (2 if dtp == BF16 else 4)
            assert nb % 4 == 0
            v = arena[:, off // 4:(off + nb) // 4]
            if dtp != F32:
                v = v.bitcast(dtp)
            if len(shape) == 3:
                v = v.rearrange("p (a b) -> p a b", a=shape[1])
            elif len(shape) == 4:
                v = v.rearrange("p (a b c) -> p a b c", a=shape[1], b=shape[2])
            return v

        XH = A(OFF_XH, [128, 8, S], BF16)
        XL = A(OFF_XL, [128, 8, S], BF16)
        ACC = A(OFF_ACC, [128, 8, S], F32)
        RING = [A(OFF_RING + i * 8192, [128, 8, 512], BF16) for i in range(4)]
        RING4 = [A(OFF_RING + i * 8192, [128, 4, 1024], BF16) for i in range(4)]
        o = OFF_SM
        def SM(shape, dtp):
            nonlocal o
            nb = int(np.prod(shape[1:])) * (2 if dtp == BF16 else 4)
            nb = (nb + 3) // 4 * 4
            v = A(o, shape, dtp)
            o += nb
            return v
        CST = SM([128, NCST], F32)
        IDB = SM([128, 128], BF16)
        MASKB = SM([128, 128], BF16)
        CMASKB = SM([128, 128], BF16)
        OM = SM([128, 512], F32)
        PF = SM([128, NF], F32)
        PFIN = SM([128, 16], F32)
        RW = SM([128, 8, 16], F32)
        RBT = SM([128, 16], F32)
        WFF = SM([128, 8, 8], BF16)
        BROW = SM([128, 512], F32)
        NGT = SM([128, 128], F32)
        BFFT = SM([128, 8], F32)
        LN_MEAN = SM([128, 512], F32)
        LN_RSTD = SM([128, 512], F32)
        LN_T = [SM([128, 512], F32), None]
        assert o <= TOTAL, (o, TOTAL)
        IDF = CST[:, 0:128]
        TI = CST[:, 128:256]
        ONES = CST[:, 256:384]
        SEL63 = CST[:, 384:512]
        A1 = CST[:, 512:640]
        A2 = CST[:, 640:768]
        CIND = CST[:, 768:772]
        DIND = CST[:, 900:902]
        R1 = OFF_ACC
        R2 = OFF_ACC + SZA
        FOXO = A(R1, [128, 4, S], BF16)
        HGO = A(R1 + SZA // 2, [128, 4, S], BF16)

        PS = [es.enter_context(nc.psum_tensor(f"ps{i}", [128, 512], F32)) for i in range(7)]
        PSB = es.enter_context(nc.psum_tensor("ps7", [128, 1024], BF16))
        engsem = {e: es.enter_context(nc.semaphore("s_" + e)) for e in ENGS}
        dkeys = ['ring0', 'ring1', 'ring2', 'ring3', 'x0', 'x1', 'pf', 'dcst', 'dpfin', 'drw', 'drbt', 'dbfft', 'wff', 'brow', 'ngt', 'lbl', 'out', 'dbg']
        dmasem = {k: es.enter_context(nc.semaphore("d_" + k)) for k in dkeys}
        block = es.enter_context(nc.Block())

        def psk(b):
            return ('ps', b)

        def mm(out, lhsT, rhs, start, stop, rd, wr):
            P.add('pe', lambda e: e.matmul(out, lhsT=lhsT, rhs=rhs, start=start, stop=stop), rd=rd, wr=wr)

        def tr(out, in_, ident, rd, wr):
            P.add('pe', lambda e: e.transpose(out=out, in_=in_, identity=ident), rd=rd, wr=wr)

        def act(out, in_, func, rd, wr, bias=None, scale=1.0):
            if bias is None:
                P.add('act', lambda e: e.activation(out=out, in_=in_, func=func, scale=scale), rd=rd, wr=wr)
            else:
                P.add('act', lambda e: e.activation(out=out, in_=in_, func=func, bias=bias, scale=scale), rd=rd, wr=wr)

        def tt(out, in0, in1, op, rd, wr, eng='dve'):
            P.add(eng, lambda e: e.tensor_tensor(out=out, in0=in0, in1=in1, op=op), rd=rd, wr=wr)

        def ts(out, in0, s1, op0, rd, wr, s2=None, op1=None, eng='dve'):
            if op1 is None:
                P.add(eng, lambda e: e.tensor_scalar(out=out, in0=in0, scalar1=s1, scalar2=None, op0=op0), rd=rd, wr=wr)
            else:
                P.add(eng, lambda e: e.tensor_scalar(out=out, in0=in0, scalar1=s1, scalar2=s2, op0=op0, op1=op1), rd=rd, wr=wr)

        def stt(out, in0, scalar, in1, op0, op1, rd, wr):
            P.add('dve', lambda e: e.scalar_tensor_tensor(out=out, in0=in0, scalar=scalar, in1=in1, op0=op0, op1=op1), rd=rd, wr=wr)

        def cp(out, in_, rd, wr, eng='dve'):
            if eng == 'act':
                P.add(eng, lambda e: e.activation(out=out, in_=in_, func=AF.Copy), rd=rd, wr=wr)
            else:
                P.add(eng, lambda e: e.tensor_copy(out=out, in_=in_), rd=rd, wr=wr)

        def memset(ap, val, wr, eng='dve'):
            P.add(eng, lambda e: e.memset(ap, val), wr=wr)

        def red(out, in_, op, rd, wr):
            P.add('dve', lambda e: e.tensor_reduce(out=out, in_=in_, op=op, axis=AX.X), rd=rd, wr=wr)

        def recip(out, in_, rd, wr):
            P.add('dve', lambda e: e.reciprocal(out=out, in_=in_), rd=rd, wr=wr)

        def dma(eng, out, in_, rd, wr, sem, nofence=False):
            return P.add(eng, lambda e: e.dma_start(out=out, in_=in_), rd=rd, wr=wr, dma=sem, nofence=nofence)

        ring_state = {'n': 0}

        def ringkeys(s):
            return [('ring', s, p) for p in range(4)]

        def slab(parts, four=False):
            s = ring_state['n'] % 4
            ring_state['n'] += 1
            view = RING4[s] if four else RING[s]
            ops = []
            for pi, (src, kc0, kcn, col0, ncols) in enumerate(parts):
                dst = view[:, kc0:kc0 + kcn, col0:col0 + ncols]
                ops.append(dma('pool', dst, src.rearrange("(c p) n -> p c n", p=128), rd=[], wr=[('ring', s, pi)], sem=f'ring{s}', nofence=True))
            for (e_, i_) in ops:
                P.q[e_][i_]['val'] = P.dma_cnt[f'ring{s}']
            return s

        dma('sp', CST, cst_d, [], ['cst'], 'dcst')
        dma('sp', PFIN, pfin_d, [], ['pfin'], 'dpfin')
        dma('sp', RW, rw_d.rearrange("p (c e) -> p c e", c=8), [], ['rw'], 'drw')
        dma('sp', RBT, rb_d.partition_broadcast(128), [], ['rbt'], 'drbt')
        cp(IDB, IDF, ['cst'], ['idb'])
        cp(MASKB, TI, ['cst'], ['maskb'])
        cp(CMASKB, CST[:, 772:900], ['cst'], ['cmaskb'])

        def xkeys(name, tti, cs=range(8)):
            return [(name, c, tti) for c in cs]

        dbg_i = {'n': 0}

        def dump_fm(name, fn_chunk, keys):
            if not dbg or name not in dbg:
                return
            k = dbg.index(name)
            for c in range(8):
                src = fn_chunk(c)
                if src is None:
                    continue
                tmp = LN_MEAN
                for t0 in range(0, S, 512):
                    cp(tmp, src[:, t0:t0 + 512], keys, ['lnmean'])
                    dma('sp', dbg_d[k, c * 128:(c + 1) * 128, t0:t0 + 512], tmp, ['lnmean'], ['dbgout'], 'dbg')

        def ln_tile(Yc, ykeys, tti, gcol, bcol, gb_key, final, seq):
            c0 = tti * 512
            tk = ['lnT0', 'lnT1']
            T = [LN_T[0], LN_RSTD]
            for c in range(8):
                mm(PS[6][:, :], ONES, Yc(c), c == 0, c == 7, ykeys(c) + ['cst'], [psk(6)])
            act(LN_MEAN, PS[6][:, :], AF.Identity, [psk(6)], ['lnmean'], scale=1.0 / D)
            for c in range(8):
                b = c % 2
                act(T[b], Yc(c), AF.Square, ykeys(c), [tk[b]])
                mm(PS[5][:, :], ONES, T[b], c == 0, c == 7, [tk[b], 'cst'], [psk(5)])
            tt(T[0], LN_MEAN, LN_MEAN, ALU.mult, ['lnmean'], [tk[0]])
            stt(LN_RSTD, PS[5][:, :], 1.0 / D, T[0], ALU.mult, ALU.subtract, [psk(5), tk[0]], [tk[1]])
            ts(LN_RSTD, LN_RSTD, LN_EPS, ALU.add, [tk[1]], [tk[1]])
            act(LN_RSTD, LN_RSTD, AF.Sqrt, [tk[1]], [tk[1]])
            recip(LN_RSTD, LN_RSTD, [tk[1]], [tk[1]])
            OUTT = A(OFF_XL, [128, 4, 1024], F32)
            for c in range(8):
                t = T[0]
                tt(t, Yc(c), LN_MEAN, ALU.subtract, ykeys(c) + ['lnmean'], [tk[0]])
                tt(t, t, LN_RSTD, ALU.mult, [tk[0], tk[1]], [tk[0]])
                act(t, t, AF.Identity, [tk[0], gb_key], [tk[0]], bias=bcol(c), scale=gcol(c))
                if not final:
                    cp(XH[:, c, c0:c0 + 512], t, [tk[0]], [('XH', c, tti)])
                    tt(XL[:, c, c0:c0 + 512], t, XH[:, c, c0:c0 + 512], ALU.subtract, [tk[0], ('XH', c, tti)], [('XL', c, tti)])
                else:
                    pb = 0 + (c % 2)
                    for q in range(4):
                        tr(PS[pb][:, q * 128:(q + 1) * 128], t[:, q * 128:(q + 1) * 128], IDF, [tk[0], 'cst'], [psk(pb)])
                    cp(OUTT[:, :, c * 128:(c + 1) * 128], PS[pb][:, :].rearrange("p (q f) -> p q f", q=4), [psk(pb)], ['outt'], eng='act' if False else 'dve')
            if final:
                dma('sp', out_d[seq, c0:c0 + 512, :].rearrange("(q p) d -> p q d", p=128), OUTT, ['outt'], ['outd'], 'out')

        import os
        STOP = os.environ.get('KSTOP', '')
        class _Stop(Exception):
            pass
        def stop_at(name):
            if STOP == name:
                raise _Stop()
        try:
          for seq in range(nseq):
              P.fence()
              XTOK = [A(R2 + i * 4096, [128, 1024], F32) for i in range(2)]
              Y0 = A(R1, [128, 8, 512], F32)
              for tti in range(NTT):
                  for q in range(4):
                      ti = tti * 4 + q
                      xb = ti % 2
                      dma('sp', XTOK[xb], x_d[seq, ti * 128:(ti + 1) * 128, :], [], [('xtok', xb)], f'x{xb}')
                      for hb in range(2):
                          for c4 in range(4):
                              c = hb * 4 + c4
                              tr(PS[hb][:, c4 * 128:(c4 + 1) * 128], XTOK[xb][:, c * 128:(c + 1) * 128], IDF, [('xtok', xb), 'cst'], [psk(hb)])
                          cp(Y0[:, hb * 4:(hb + 1) * 4, q * 128:(q + 1) * 128], PS[hb][:, :].rearrange("p (c t) -> p c t", c=4), [psk(hb)], [('y0', hb)],
                             eng='dve')
                  ln_tile(lambda c: Y0[:, c, :], lambda c: [('y0', c // 4)], tti, lambda c: PFIN[:, c:c + 1], lambda c: PFIN[:, 8 + c:9 + c], 'pfin', False, seq)
              if seq == 0:
                  dump_fm('ln_in', lambda c: XH[:, c, :], [k for t_ in range(NTT) for k in xkeys('XH', t_)])
              stop_at('p0')

              for li, l in enumerate(layers):
                  last = (li == len(layers) - 1)
                  P.fence()
                  dma('sp', PF, pf_d[l], [], ['pf'], 'pf')
                  BQK = lambda c: PF[:, c:c + 1]
                  BFV64 = PF[0:64, 68:76]
                  if l == 0:
                      memset(OM, 1.0, ['om'])
                  else:
                      LBT = A(R2, [128, 4, 512], F32)
                      LBM = A(R2 + 8192, [128, 512], F32)
                      LBS = A(R2 + 8192 + 2048, [128, 512], F32)
                      dma('sp', LBT, lbl_d.partition_broadcast(128), [], ['lbt'], 'lbl')
                      tt(LBM, LBT[:, 0, :], LBT[:, 1, :], ALU.max, ['lbt'], ['lbm'])
                      tt(LBM, LBM, LBT[:, 2, :], ALU.max, ['lbt', 'lbm'], ['lbm'])
                      tt(LBM, LBM, LBT[:, 3, :], ALU.max, ['lbt', 'lbm'], ['lbm'])
                      for r in range(4):
                          tt(LBT[:, r, :], LBT[:, r, :], LBM, ALU.subtract, ['lbt', 'lbm'], ['lbt'])
                      act(LBT, LBT, AF.Exp, ['lbt'], ['lbt'])
                      tt(LBS, LBT[:, 0, :], LBT[:, 1, :], ALU.add, ['lbt'], ['lbs'])
                      tt(LBS, LBS, LBT[:, 2, :], ALU.add, ['lbt', 'lbs'], ['lbs'])
                      tt(LBS, LBS, LBT[:, 3, :], ALU.add, ['lbt', 'lbs'], ['lbs'])
                      recip(LBS, LBS, ['lbs'], ['lbs'])
                      cp(LBM, LBT[:, 1, :], ['lbt'], ['lbm'])
                      for r in range(2, l + 1):
                          tt(LBM, LBM, LBT[:, r, :], ALU.add, ['lbt', 'lbm'], ['lbm'])
                      tt(LBM, LBM, LBS, ALU.mult, ['lbm', 'lbs'], ['lbm'])
                      ts(OM, LBM, -1.0, ALU.mult, ['lbm'], ['om'], s2=1.0, op1=ALU.add)
                  P.fence()

                  o2 = R2
                  QT = A(o2, [128, S], BF16); o2 += 2 * S
                  KT = A(o2, [128, S], BF16); o2 += 2 * S
                  VP = A(o2, [128, NT, 2, 66], BF16); o2 += NT * 2 * 66 * 2
                  CB = A(o2, [128, 2, NT, NT], F32); o2 += 2 * NT * NT * 4
                  SPT = A(o2, [128, NT * 8], F32); o2 += NT * 8 * 4
                  GTOK = A(o2, [128, NT, 8], F32); o2 += NT * 8 * 4
                  GREF = A(o2, [128, NT, 8], F32); o2 += NT * 8 * 4
                  OFFT = A(o2, [128, NT, 8], F32); o2 += NT * 8 * 4
                  TOTT = A(o2, [128, NT, 8], F32); o2 += NT * 8 * 4
                  PT = [A(o2 + i * 1024, [128, 512], BF16) for i in range(2)]; o2 += 2048
                  OTMP = A(o2, [128, 512], F32); o2 += 2048
                  RSB = A(o2, [128, 512], F32); o2 += 2048
                  RS = A(o2, [128, 512], F32); o2 += 2048
                  assert o2 <= R2 + SZA, (o2 - R2, SZA)

                  dma('pool', WFF, win_d[l][:, 1536:1544].rearrange("(c p) n -> p c n", p=128), [], ['wff'], 'wff')
                  dma('sp', BFFT, pr_d[l, :, 0:8].partition_broadcast(128), [], ['bfft'], 'dbfft')
                  allxh = [k for t_ in range(NTT) for k in xkeys('XH', t_)]
                  for i in range(NT):
                      mm(PS[6][:, i * 8:(i + 1) * 8], CST[0:1, 256:384], BFFT[0:1, :], True, False, ['cst', 'bfft'], [psk(6)])
                      for k in range(8):
                          mm(PS[6][:, i * 8:(i + 1) * 8], XH[:, k, i * 128:(i + 1) * 128], WFF[:, k, :], False, k == 7, [('XH', k, i // 4), 'wff'], [psk(6)])
                  act(SPT, PS[6][:, 0:NT * 8], AF.Exp, [psk(6)], ['spt'], scale=-1.0)
                  act(SPT, SPT, AF.Ln, ['spt'], ['spt'], bias=1.0, scale=1.0)
                  mm(PS[6][:, 0:NT * 8], TI, SPT, True, True, ['cst', 'spt'], [psk(6)])
                  mm(PS[5][:, 0:NT * 8], ONES, SPT, True, True, ['cst', 'spt'], [psk(5)])
                  cp(TOTT, PS[5][:, 0:NT * 8].rearrange("p (i h) -> p i h", h=8), [psk(5)], ['tott'])
                  memset(OFFT[:, 0, :], 0.0, ['offt'])
                  for i in range(1, NT):
                      tt(OFFT[:, i, :], OFFT[:, i - 1, :], TOTT[:, i - 1, :], ALU.add, ['offt', 'tott'], ['offt'])
                  tt(GTOK, PS[6][:, 0:NT * 8].rearrange("p (i h) -> p i h", h=8), OFFT, ALU.add, [psk(6), 'offt'], ['gtok'])
                  mm(PS[6][:, 0:NT * 8], SEL63, GTOK.rearrange("p i h -> p (i h)"), True, True, ['cst', 'gtok'], [psk(6)])
                  act(GREF, PS[6][:, 0:NT * 8].rearrange("p (i h) -> p i h", h=8), AF.Copy, [psk(6)], ['gref'])

                  stop_at('pF')
                  sti = 0
                  oi = 0
                  for j in range(4):
                      s = slab([(win_d[l][:, j * 128:(j + 1) * 128], 0, 8, 0, 128),
                                (win_d[l][:, 512 + j * 128:512 + (j + 1) * 128], 0, 8, 128, 128),
                                (win_d[l][:, 1024 + j * 128:1024 + (j + 1) * 128], 0, 8, 256, 128)])
                      W = RING[s]
                      rk = ringkeys(s)
                      for hp in range(2):
                          h = 2 * j + hp
                          tt(CB[:, hp, :, :], GTOK[:, :, h].unsqueeze(1).broadcast_to([128, NT, NT]), GREF[:, :, h].unsqueeze(2).broadcast_to([128, NT, NT]),
                             ALU.subtract, ['gtok', 'gref'], ['cb'])
                      for tti in range(NTT):
                          sl = slice(tti * 512, (tti + 1) * 512)
                          for wh, dst, nm in ((0, QT, 'qt'), (1, KT, 'kt')):
                              pb = sti % 4; sti += 1
                              for k in range(8):
                                  mm(PS[pb][:, :], W[:, k, wh * 128:(wh + 1) * 128], XH[:, k, sl], k == 0, k == 7, rk + [('XH', k, tti)], [psk(pb)])
                              act(dst[:, sl], PS[pb][:, :], AF.Identity, [psk(pb), 'pf'], [nm], bias=BQK(wh * 4 + j))
                      memset(VP[:, :, :, 64:65], 1.0, ['vp'])
                      for g in range(NTT):
                          pb = sti % 4; sti += 1
                          for q in range(4):
                              i = g * 4 + q
                              for k in range(8):
                                  mm(PS[pb][:, q * 128:(q + 1) * 128], XH[:, k, i * 128:(i + 1) * 128], W[:, k, 256:384], k == 0, k == 7, rk + [('XH', k, g)], [psk(pb)])
                          cp(VP[:, g * 4:(g + 1) * 4, :, 0:64], PS[pb][:, :].rearrange("p (q h e) -> p q h e", q=4, h=2), [psk(pb)], ['vp'])
                      for hp in range(2):
                          h = 2 * j + hp
                          hs = slice(hp * 64, (hp + 1) * 64)
                          for Q in range(NTT):
                              q0 = Q * 512
                              ob = 4 + (oi % 2); oi += 1
                              nk = Q * 4 + 4
                              for kt in range(nk):
                                  qlo = max(kt * 128, q0)
                                  cl = qlo - q0
                                  pb = sti % 4; sti += 1
                                  ptb = sti % 2
                                  mm(PS[pb][:, cl:512], KT[hs, kt * 128:(kt + 1) * 128], QT[hs, qlo:q0 + 512], True, True, ['kt', 'qt'], [psk(pb)])
                                  for qt in range(qlo // 128, Q * 4 + 4):
                                      c_ = qt * 128 - q0
                                      act(PT[ptb][:, c_:c_ + 128], PS[pb][:, c_:c_ + 128], AF.Exp, [psk(pb), 'cb'], [('pt', ptb)], bias=CB[:, hp, qt, kt:kt + 1], scale=0.125)
                                  if kt * 128 >= q0:
                                      tt(PT[ptb][:, cl:cl + 128], PT[ptb][:, cl:cl + 128], MASKB, ALU.mult, [('pt', ptb), 'maskb'], [('pt', ptb)])
                                  mm(PS[ob][0:65, cl:512], VP[:, kt, hp, 0:65], PT[ptb][:, cl:512], kt == 0, kt == nk - 1, ['vp', ('pt', ptb)], [psk(ob)])
                              recip(RS[64:65, :], PS[ob][64:65, :], [psk(ob)], ['rs'])
                              mm(PS[6][0:64, :], CST[64:65, 256:320], RS[64:65, :], True, True, ['cst', 'rs'], [psk(6)])
                              act(RSB[0:64, :], PS[6][0:64, :], AF.Copy, [psk(6)], ['rsb'])
                              tt(OTMP[0:64, :], PS[ob][0:64, :], RSB[0:64, :], ALU.mult, [psk(ob), 'rsb'], ['otmp'])
                              ts(FOXO[hs, j, q0:q0 + 512], OTMP[0:64, :], BFV64[:, h:h + 1], ALU.add, ['otmp', 'pf'], [('foxo', j)])
                  dump_fm('foxo', lambda c: FOXO[:, c, :] if c < 4 else None, [('foxo', j) for j in range(4)])
                  stop_at('fox')
                  P.fence()

                  o2 = R2
                  def T2(shape, dtp, n=2):
                      nonlocal o2
                      r = []
                      for _ in range(n):
                          nb = int(np.prod(shape[1:])) * (2 if dtp == BF16 else 4)
                          r.append(A(o2, shape, dtp)); o2 += nb
                      return r
                  QF = T2([128, 128], F32); SIG = T2([128, 128], F32); KF = T2([128, 128], F32); LF = T2([128, 128], F32)
                  VB = T2([128, 128], BF16); GO = T2([128, 128], F32)
                  EU = T2([128, 128], F32); ENU = T2([128, 128], F32); EW = T2([128, 128], F32); DEC = T2([128, 4], F32)
                  QTL = T2([128, 128], BF16); KTL = T2([128, 128], BF16); KH = T2([128, 128], BF16)
                  QTT = T2([128, 128], BF16); KTT = T2([128, 128], BF16)
                  OF = T2([128, 128], F32); OSQ = T2([128, 128], F32); SS = T2([128, 2], F32); OG = T2([128, 128], BF16)
                  QX = [T2([128, 128], BF16) for _ in range(2)]; KX = [T2([128, 128], BF16) for _ in range(2)]; LX = [T2([128, 128], F32) for _ in range(2)]
                  QBD = [T2([128, 128], BF16) for _ in range(2)]; DECH = T2([128, 4], F32)
                  SMK = [T2([128, 128], BF16) for _ in range(2)]
                  STS = T2([128, 64], F32); SSB = T2([128, 64], BF16)
                  assert o2 <= R2 + SZA
                  for j in range(4):
                      s = slab([(win_d[l][:, 1544 + sl_ * 512 + j * 128:1544 + sl_ * 512 + (j + 1) * 128], 0, 8, sl_ * 128, 128) for sl_ in range(4)])
                      W = RING[s]
                      rk = ringkeys(s)
                      dma('sp', BROW, pr_d[l, :, 8 + j * 512:8 + (j + 1) * 512].partition_broadcast(128), [], ['brow'], 'brow')
                      dma('sp', NGT, pr_d[l, :, 2056 + j * 128:2056 + (j + 1) * 128].partition_broadcast(128), [], ['ngt'], 'ngt')
                      for hp in range(2):
                          memset(STS[hp], 0.0, [('ss', hp)])
                      OMJ = OM[:, j * 128:(j + 1) * 128]
                      for i in range(NT):
                          b = i % 2
                          K = lambda n: (n, b)
                          tsl = slice(i * 128, (i + 1) * 128)
                          pa = 0 + b
                          mm(PS[pa][:, :], CST[0:1, 256:384], BROW[0:1, :], True, False, ['cst', 'brow'], [psk(pa)])
                          for k in range(8):
                              mm(PS[pa][:, :], XH[:, k, tsl], W[:, k, :], False, k == 7, rk + [('XH', k, i // 4)], [psk(pa)])
                          act(QF[b], PS[pa][:, 0:128], AF.Silu, [psk(pa)], [K('qf')])
                          act(SIG[b], PS[pa][:, 128:256], AF.Sigmoid, [psk(pa)], [K('sig')])
                          act(VB[b], PS[pa][:, 256:384], AF.Copy, [psk(pa)], [K('vb')])
                          act(GO[b], PS[pa][:, 384:512], AF.Silu, [psk(pa)], [K('go')])
                          ts(SIG[b], SIG[b], -1.0, ALU.mult, [K('sig')], [K('sig')], s2=1.0, op1=ALU.add)
                          tt(KF[b], SIG[b], OMJ, ALU.mult, [K('sig'), 'om'], [K('kf')])
                          ts(LF[b], KF[b], -1.0, ALU.mult, [K('kf')], [K('lf')], s2=1.0, op1=ALU.add)
                          act(LF[b], LF[b], AF.Ln, [K('lf')], [K('lf')])
                          pbk = 2 + b
                          mm(PS[pbk][:, 0:128], A1, LF[b], True, True, ['cst', K('lf')], [psk(pbk)])
                          mm(PS[pbk][:, 128:256], A2, LF[b], True, True, ['cst', K('lf')], [psk(pbk)])
                          act(EU[b], PS[pbk][:, 0:128], AF.Exp, [psk(pbk)], [K('eu')])
                          act(ENU[b], PS[pbk][:, 0:128], AF.Exp, [psk(pbk)], [K('enu')], scale=-1.0)
                          act(EW[b], PS[pbk][:, 128:256], AF.Exp, [psk(pbk)], [K('ew')])
                          tt(QTL[b], QF[b], EU[b], ALU.mult, [K('qf'), K('eu')], [K('qtl')])
                          tt(KTL[b], KF[b], ENU[b], ALU.mult, [K('kf'), K('enu')], [K('ktl')])
                          tt(KH[b], KF[b], EW[b], ALU.mult, [K('kf'), K('ew')], [K('kh')])
                          tr(PSB[:, 0:128], QTL[b], IDB, [K('qtl'), 'idb'], [psk(7)])
                          tr(PSB[:, 128:256], KTL[b], IDB, [K('ktl'), 'idb'], [psk(7)])
                          cp(QTT[b], PSB[:, 0:128], [psk(7)], [K('qtt')])
                          cp(KTT[b], PSB[:, 128:256], [psk(7)], [K('ktt')])
                          stop_at('h3')
                          for hp in range(2):
                              hs = slice(hp * 64, (hp + 1) * 64)
                              for c2 in range(2):
                                  cc = slice(c2 * 64, (c2 + 1) * 64)
                                  ts(QX[b][hp][:, cc], QTL[b][:, hs], CST[:, 768 + c2:769 + c2], ALU.mult, [K('qtl'), 'cst'], [('qx', hp, b)])
                                  ts(KX[b][hp][:, cc], KH[b][:, hs], CST[:, 768 + c2:769 + c2], ALU.mult, [K('kh'), 'cst'], [('kx', hp, b)])
                                  ts(LX[b][hp][:, cc], LF[b][:, hs], CST[:, 768 + c2:769 + c2], ALU.mult, [K('lf'), 'cst'], [('lx', hp, b)])
                              tr(PSB[:, 256 + hp * 128:384 + hp * 128], QX[b][hp], IDB, [('qx', hp, b), 'idb'], [psk(7)])
                              cp(QBD[b][hp], PSB[:, 256 + hp * 128:384 + hp * 128], [psk(7)], [('qbd', hp, b)])
                              mm(PS[4 + hp][:, 0:128], KTT[b][hs, :], QTT[b][hs, :], True, True, [K('ktt'), K('qtt')], [psk(4 + hp)])
                              tt(SMK[b][hp], PS[4 + hp][:, 0:128], CMASKB, ALU.mult, [psk(4 + hp), 'cmaskb'], [('smk', hp, b)])
                              mm(PS[6][:, hs], KX[b][hp], VB[b][:, hs], True, True, [('kx', hp, b), K('vb')], [psk(6)])
                          pbk = 2 + b
                          for hp in range(2):
                              mm(PS[pbk][:, 384 + hp * 2:386 + hp * 2], LX[b][hp], DIND, True, True, [('lx', hp, b), 'cst'], [psk(pbk)])
                          act(DECH[b], PS[pbk][:, 384:388], AF.Exp, [psk(pbk)], [K('dech')])
                          for hp in range(2):
                              hs = slice(hp * 64, (hp + 1) * 64)
                              stt(STS[hp][64:128, :], STS[hp][0:64, :], DECH[b][0:64, hp * 2:hp * 2 + 1], PS[6][0:64, hs], ALU.mult, ALU.add, [('ss', hp), K('dech'), psk(6)], [('ss', hp)])
                              ts(SSB[hp], STS[hp], DECH[b][:, hp * 2 + 1:hp * 2 + 2], ALU.mult, [('ss', hp), K('dech')], [('ssb', hp)])
                              stt(STS[hp][0:64, :], STS[hp][64:128, :], DECH[b][64:128, hp * 2:hp * 2 + 1], PS[6][64:128, hs], ALU.mult, ALU.add, [('ss', hp), K('dech'), psk(6)], [('ss', hp)])
                          for hp in range(2):
                              hs = slice(hp * 64, (hp + 1) * 64)
                              mm(PS[pbk][:, 256 + hp * 64:320 + hp * 64], SMK[b][hp], VB[b][:, hs], True, False, [('smk', hp, b), K('vb')], [psk(pbk)])
                              mm(PS[pbk][:, 256 + hp * 64:320 + hp * 64], QBD[b][hp], SSB[hp], False, True, [('qbd', hp, b), ('ssb', hp)], [psk(pbk)])
                          act(OF[b], PS[pbk][:, 256:384], AF.Copy, [psk(pbk)], [K('of')])
                          tt(OSQ[b], OF[b], OF[b], ALU.mult, [K('of')], [K('osq')])
                          red(SS[b], OSQ[b].rearrange("p (h e) -> p h e", h=2), ALU.add, [K('osq')], [K('ss')])
                          act(SS[b], SS[b], AF.Sqrt, [K('ss')], [K('ss')], bias=RMS_EPS, scale=1.0 / 64)
                          recip(SS[b], SS[b], [K('ss')], [K('ss')])
                          tt(GO[b], GO[b], NGT, ALU.mult, [K('go'), 'ngt'], [K('go')])
                          tt(OF[b].rearrange("p (h e) -> p h e", h=2), OF[b].rearrange("p (h e) -> p h e", h=2), SS[b].unsqueeze(2).broadcast_to([128, 2, 64]),
                             ALU.mult, [K('of'), K('ss')], [K('of')])
                          tt(OG[b], OF[b], GO[b], ALU.mult, [K('of'), K('go')], [K('og')])
                          tr(PSB[:, 512:640], OG[b], IDB, [K('og'), 'idb'], [psk(7)])
                          cp(HGO[:, j, tsl], PSB[:, 512:640], [psk(7)], [('hgo', j)])
                  dump_fm('hgo', lambda c: HGO[:, c, :] if c < 4 else None, [('hgo', j) for j in range(4)])
                  stop_at('hgrn')
                  P.fence()

                  MIXIN = A(R2, [128, 8, 512], BF16)
                  YM = A(R2 + 8192, [128, 8, 512], F32)
                  TG = [A(R2 + 8192 + 16384 + i * 1024, [128, 512], BF16) for i in range(2)]
                  TM = [A(R2 + 8192 + 16384 + 2048 + i * 2048, [128, 512], F32) for i in range(2)]
                  fk = [('foxo', j) for j in range(4)]
                  hk = [('hgo', j) for j in range(4)]
                  for tti in range(NTT):
                      sl = slice(tti * 512, (tti + 1) * 512)
                      for half in range(2):
                          s1 = slab([(win_d[l][:, 3592 + half * 512:3592 + (half + 1) * 512], 0, 8, 0, 512)])
                          s2 = slab([(win_d[l][:, 4616 + half * 512:4616 + (half + 1) * 512], 0, 8, 0, 512)])
                          s3 = slab([(wfb_d[l][:, half * 512:(half + 1) * 512], 0, 4, 0, 512), (whb_d[l][:, half * 512:(half + 1) * 512], 4, 4, 0, 512)])
                          for fc in range(4):
                              c = half * 4 + fc
                              fs = slice(fc * 128, (fc + 1) * 128)
                              for k in range(8):
                                  mm(PS[0][:, :], RING[s1][:, k, fs], XH[:, k, sl], k == 0, k == 7, ringkeys(s1) + [('XH', k, tti)], [psk(0)])
                              act(TG[0], PS[0][:, :], AF.Sigmoid, [psk(0), 'pf'], ['tg0'], bias=PF[:, 12 + c:13 + c])
                              for k in range(8):
                                  mm(PS[1][:, :], RING[s2][:, k, fs], XH[:, k, sl], k == 0, k == 7, ringkeys(s2) + [('XH', k, tti)], [psk(1)])
                              act(TG[1], PS[1][:, :], AF.Sigmoid, [psk(1), 'pf'], ['tg1'], bias=PF[:, 20 + c:21 + c])
                              for k in range(4):
                                  mm(PS[2][:, :], RING[s3][:, k, fs], FOXO[:, k, sl], k == 0, k == 3, ringkeys(s3) + fk, [psk(2)])
                              tt(TM[0], PS[2][:, :], TG[0], ALU.mult, [psk(2), 'tg0'], ['tm0'])
                              for k in range(4):
                                  mm(PS[3][:, :], RING[s3][:, 4 + k, fs], HGO[:, k, sl], k == 0, k == 3, ringkeys(s3) + hk, [psk(3)])
                              tt(TM[1], PS[3][:, :], TG[1], ALU.mult, [psk(3), 'tg1'], ['tm1'])
                              tt(MIXIN[:, c, :], TM[0], TM[1], ALU.add, ['tm0', 'tm1'], [('mixin', c)])
                      sm_ = [slab([(wmx_d[l][:, hf * 512:(hf + 1) * 512], 0, 8, 0, 512)]) for hf in range(2)]
                      for c in range(8):
                          s = sm_[c // 4]
                          fs = slice((c % 4) * 128, (c % 4 + 1) * 128)
                          pb = 4 + (c % 2)
                          for k in range(8):
                              mm(PS[pb][:, :], RING[s][:, k, fs], MIXIN[:, k, :], k == 0, k == 7, ringkeys(s) + [('mixin', k)], [psk(pb)])
                          act(YM[:, c, :], PS[pb][:, :], AF.Identity, [psk(pb), 'pf'], [('ym', c)], bias=PF[:, 28 + c:29 + c])
                          stt(YM[:, c, :], XH[:, c, sl], ALPHA, YM[:, c, :], ALU.mult, ALU.add, [('XH', c, tti), ('ym', c)], [('ym', c)])
                          stt(YM[:, c, :], XL[:, c, sl], ALPHA, YM[:, c, :], ALU.mult, ALU.add, [('XL', c, tti), ('ym', c)], [('ym', c)])
                      ln_tile(lambda c: YM[:, c, :], lambda c: [('ym', c)], tti, lambda c: PF[:, 36 + c:37 + c], lambda c: PF[:, 44 + c:45 + c], 'pf', False, seq)
                  if seq == 0:
                      dump_fm('ln1', lambda c: XH[:, c, :], [k for t_ in range(NTT) for k in xkeys('XH', t_)])
                  stop_at('merge')
                  P.fence()

                  for c in range(8):
                      ts(ACC[:, c, :], XH[:, c, :], ALPHA, ALU.mult, [('XH', c, t_) for t_ in range(NTT)], [('ACC', c, t_) for t_ in range(NTT)])
                      stt(ACC[:, c, :], XL[:, c, :], ALPHA, ACC[:, c, :], ALU.mult, ALU.add, [('XL', c, t_) for t_ in range(NTT)] + [('ACC', c, t_) for t_ in range(NTT)],
                          [('ACC', c, t_) for t_ in range(NTT)])
                  P.fence()
                  o3 = OFF_XL
                  def T3(shape, dtp):
                      nonlocal o3
                      nb = int(np.prod(shape[1:])) * (2 if dtp == BF16 else 4)
                      v = A(o3, shape, dtp); o3 += (nb + 3) // 4 * 4
                      return v
                  GT = T3([128, S], F32)
                  HT = [T3([128, 4, 512], BF16) for _ in range(2)]
                  SA = [T3([128, 512], BF16) for _ in range(2)]
                  TB = [T3([128, 512], BF16) for _ in range(2)]
                  GSB = T3([128, 512], F32)
                  RE = T3([128, 512], F32)
                  L_ = T3([128, NT, 16], F32); LM = T3([128, NT, 16], F32); IS1 = T3([128, NT, 16], F32); IS2 = T3([128, NT, 16], F32)
                  GG = T3([128, NT, 16], F32)
                  GMX = T3([128, NT, 4], F32); GMK = T3([128, NT, 4], F32); GT1 = T3([128, NT, 4], F32)
                  BMX = T3([128, NT], F32); M2 = T3([128, NT], F32); G1 = T3([128, NT], F32); G2 = T3([128, NT], F32)
                  assert o3 <= OFF_XL + SZA, (o3 - OFF_XL, SZA)
                  for i in range(NT):
                      for k in range(8):
                          mm(PS[6][:, i * 16:(i + 1) * 16], ACC[:, k, i * 128:(i + 1) * 128], RW[:, k, :], k == 0, k == 7, [('ACC', k, i // 4), 'rw'], [psk(6)])
                  stt(L_, PS[6][:, 0:NT * 16].rearrange("p (i e) -> p i e", e=16), 1.0 / ALPHA, RBT.unsqueeze(1).broadcast_to([128, NT, 16]), ALU.mult, ALU.add,
                      [psk(6), 'rbt'], ['L'])
                  red(GMX.rearrange("p i g -> p (i g)"), L_.rearrange("p i (g e) -> p (i g) e", g=4), ALU.max, ['L'], ['gmx'])
                  red(BMX, GMX, ALU.max, ['gmx'], ['bmx'])
                  tt(GMK, GMX, BMX.unsqueeze(2).broadcast_to([128, NT, 4]), ALU.is_equal, ['gmx', 'bmx'], ['gmk'])
                  ts(GT1, GMK, BIG, ALU.mult, ['gmk'], ['gt1'], s2=-BIG, op1=ALU.add)
                  L4 = lambda t_: t_.rearrange("p i (g e) -> p (i g) e", g=4)
                  G4 = lambda t_: t_.rearrange("p i g -> p (i g)").unsqueeze(2).broadcast_to([128, NT * 4, 4])
                  tt(L4(LM), L4(L_), G4(GMK), ALU.mult, ['L', 'gmk'], ['lm'])
                  tt(L4(LM), L4(LM), G4(GT1), ALU.add, ['lm', 'gt1'], ['lm'])
                  tt(IS1, LM, BMX.unsqueeze(2).broadcast_to([128, NT, 16]), ALU.is_equal, ['lm', 'bmx'], ['is1'])
                  stt(LM, IS1, -BIG, LM, ALU.mult, ALU.add, ['is1', 'lm'], ['lm'])
                  red(M2, LM, ALU.max, ['lm'], ['m2'])
                  tt(IS2, LM, M2.unsqueeze(2).broadcast_to([128, NT, 16]), ALU.is_equal, ['lm', 'm2'], ['is2'])
                  tt(G1, BMX, M2, ALU.subtract, ['bmx', 'm2'], ['g1'])
                  act(G1, G1, AF.Sigmoid, ['g1'], ['g1'])
                  ts(G2, G1, -1.0, ALU.mult, ['g1'], ['g2'], s2=1.0, op1=ALU.add)
                  tt(IS1, IS1, G1.unsqueeze(2).broadcast_to([128, NT, 16]), ALU.mult, ['is1', 'g1'], ['is1'])
                  tt(IS2, IS2, G2.unsqueeze(2).broadcast_to([128, NT, 16]), ALU.mult, ['is2', 'g2'], ['is2'])
                  tt(GG, IS1, IS2, ALU.add, ['is1', 'is2'], ['gg'])
                  for g in range(NTT):
                      for q in range(4):
                          i = g * 4 + q
                          tr(PS[6][0:16, q * 128:(q + 1) * 128], GG[:, i, :], IDF, ['gg', 'cst'], [psk(6)])
                      act(GT[0:16, g * 512:(g + 1) * 512], PS[6][0:16, :], AF.Copy, [psk(6)], ['gt'])
                  ei = 0
                  for e in range(NE):
                      for half in range(2):
                          sa_ = slab([(w1_d[l, e][:, half * 512:(half + 1) * 512], 0, 8, 0, 512)])
                          sb_ = slab([(w3_d[l, e][:, half * 512:(half + 1) * 512], 0, 8, 0, 512)])
                          sc_ = slab([(w2_d[l, e][half * 512:(half + 1) * 512, :], 0, 4, 0, 1024)], four=True)
                          for tti in range(NTT):
                              sl = slice(tti * 512, (tti + 1) * 512)
                              hb = ei % 2; ei += 1
                              ts(RE[0:16, :], GT[0:16, sl], IDF[0:16, e:e + 1], ALU.mult, ['gt', 'cst'], ['re'])
                              mm(PS[6][:, :], ONES[0:16, :], RE[0:16, :], True, True, ['cst', 're'], [psk(6)])
                              act(GSB, PS[6][:, :], AF.Copy, [psk(6)], ['gsb'])
                              for hc in range(4):
                                  fs = slice(hc * 128, (hc + 1) * 128)
                                  b2 = hc % 2
                                  for k in range(8):
                                      mm(PS[b2][:, :], RING[sa_][:, k, fs], XH[:, k, sl], k == 0, k == 7, ringkeys(sa_) + [('XH', k, tti)], [psk(b2)])
                                  act(SA[b2], PS[b2][:, :], AF.Silu, [psk(b2)], [('sa', b2)])
                                  for k in range(8):
                                      mm(PS[2 + b2][:, :], RING[sb_][:, k, fs], XH[:, k, sl], k == 0, k == 7, ringkeys(sb_) + [('XH', k, tti)], [psk(2 + b2)])
                                  tt(TB[b2], PS[2 + b2][:, :], GSB, ALU.mult, [psk(2 + b2), 'gsb'], [('tb', b2)])
                                  tt(HT[hb][:, hc, :], SA[b2], TB[b2], ALU.mult, [('sa', b2), ('tb', b2)], [('ht', hb, hc)])
                              for fc in range(8):
                                  pb = 4 + (fc % 2)
                                  for hc in range(4):
                                      mm(PS[pb][:, :], RING4[sc_][:, hc, fc * 128:(fc + 1) * 128], HT[hb][:, hc, :], hc == 0, hc == 3, ringkeys(sc_) + [('ht', hb, hc)], [psk(pb)])
                                  tt(ACC[:, fc, sl], PS[pb][:, :], ACC[:, fc, sl], ALU.add, [psk(pb), ('ACC', fc, tti)], [('ACC', fc, tti)])
                  P.fence()
                  for tti in range(NTT):
                      sl = slice(tti * 512, (tti + 1) * 512)
                      ln_tile(lambda c: ACC[:, c, sl], lambda c: [('ACC', c, tti)], tti, lambda c: PF[:, 52 + c:53 + c], lambda c: PF[:, 60 + c:61 + c], 'pf', last, seq)
                  if seq == 0 and not last:
                      dump_fm('ln2', lambda c: XH[:, c, :], [k for t_ in range(NTT) for k in xkeys('XH', t_)])


        except _Stop:
            pass
        P.finalize()
        block.tensor(P.body('pe', engsem, dmasem))
        block.scalar(P.body('act', engsem, dmasem))
        block.vector(P.body('dve', engsem, dmasem))
        block.gpsimd(P.body('pool', engsem, dmasem))
        block.sync(P.body('sp', engsem, dmasem))
    return nc, P


def prep_inputs(inp):
    f = lambda a: np.ascontiguousarray(np.asarray(a, dtype=np.float32))
    b_in = f(inp['b_in'])
    L = b_in.shape[0]
    fm = lambda v: v.reshape(-1, 128).T
    pf = np.zeros((L, 128, NF), np.float32)
    pr = np.zeros((L, 1, NR), np.float32)
    for l in range(L):
        pf[l, :, 0:8] = fm(b_in[l, 0:1024])
        pf[l, :, 8:12] = fm(b_in[l, 1024:1536])
        pf[l, :, 12:28] = fm(b_in[l, 3592:5640])
        pf[l, :, 28:36] = fm(f(inp['b_mix_out'])[l])
        pf[l, :, 36:44] = fm(f(inp['ln1_g'])[l])
        pf[l, :, 44:52] = fm(f(inp['ln1_b'])[l])
        pf[l, :, 52:60] = fm(f(inp['ln2_g'])[l])
        pf[l, :, 60:68] = fm(f(inp['ln2_b'])[l])
        pf[l, 0:64, 68:76] = b_in[l, 1024:1536].reshape(8, 64).T
        pr[l, 0, 0:8] = b_in[l, 1536:1544]
        for j in range(4):
            for s in range(4):
                pr[l, 0, 8 + j * 512 + s * 128:8 + j * 512 + (s + 1) * 128] = b_in[l, 1544 + s * 512 + j * 128:1544 + s * 512 + (j + 1) * 128]
        pr[l, 0, 2056:2568] = f(inp['hgrn_norm_g'])[l]
    pfin = np.concatenate([fm(f(inp['ln_in_g'])), fm(f(inp['ln_in_b']))], axis=1)
    rw = f(inp['router_w']).reshape(8, 128, 16).transpose(1, 0, 2).reshape(128, 128)
    shared = {
        'w_in': f(inp['w_in']), 'w_fb': f(inp['w_fox_branch']), 'w_hb': f(inp['w_hgrn_branch']), 'w_mix': f(inp['w_mix_out']),
        'w1': f(inp['expert_w1']), 'w3': f(inp['expert_w3']), 'w2': f(inp['expert_w2']),
        'pf': pf, 'pfin': np.ascontiguousarray(pfin), 'pr': pr, 'lbl': f(inp['hgrn_lb_logits']).reshape(1, -1),
        'rw': np.ascontiguousarray(rw), 'rb': f(inp['router_b']).reshape(1, 16), 'cst': make_consts(),
    }
    return shared


_CACHE = {}


def kernel(**inputs):
    x = np.ascontiguousarray(np.asarray(inputs['x'], dtype=np.float32))
    B, S, _ = x.shape
    ncores = 8
    nseq = B // ncores
    depth = np.asarray(inputs['w_in']).shape[0]
    key = (nseq, S, depth)
    if key not in _CACHE:
        _CACHE[key] = build(nseq, S, list(range(depth)))[0]
    nc = _CACHE[key]
    shared = prep_inputs(inputs)
    in_maps = []
    for c in range(ncores):
        m = dict(shared)
        m['x'] = np.ascontiguousarray(x[c * nseq:(c + 1) * nseq])
        in_maps.append(m)
    res = run_bass_kernel_spmd(nc, in_maps, core_ids=list(range(ncores)))
    return np.concatenate([r['out'] for r in res.results], axis=0).astype(np.float32)
```

```python
import numpy as np
from contextlib import ExitStack
import concourse.bass as bass
import concourse.mybir as mybir
from concourse.bass_utils import run_bass_kernel_spmd

F32 = mybir.dt.float32
BF16 = mybir.dt.bfloat16
AF = mybir.ActivationFunctionType
ALU = mybir.AluOpType
AX = mybir.AxisListType

D = 1024
NIN = 5640
NE = 16
ALPHA = float(8 ** 0.25)
LN_EPS = 1e-5
RMS_EPS = 1e-6
BIG = 1.0e4
ENGS = ('pe', 'act', 'dve', 'pool', 'sp')
NF = 76
NR = 8 + 2048 + 512
NCST = 902


class Prog:
    def __init__(self):
        self.q = {e: [] for e in ENGS}
        self.lw = {}
        self.rd = {}
        self.dma_cnt = {}
        self.fence_op = None

    def add(self, eng, fn, rd=(), wr=(), dma=None, nofence=False):
        idx = len(self.q[eng])
        me = (eng, idx)
        deps = set()
        for k in rd:
            w = self.lw.get(k)
            if w is not None:
                deps.add(w)
        for k in wr:
            w = self.lw.get(k)
            if w is not None:
                deps.add(w)
            for r in self.rd.get(k, ()):
                deps.add(r)
        if self.fence_op is not None and not nofence:
            deps.add(self.fence_op)
        deps.discard(me)
        for k in rd:
            self.rd.setdefault(k, []).append(me)
        for k in wr:
            self.lw[k] = me
            self.rd[k] = []
        rec = dict(fn=fn, deps=deps, dma=dma, inc=False, val=None)
        if dma is not None:
            self.dma_cnt[dma] = self.dma_cnt.get(dma, 0) + 16
            rec['val'] = self.dma_cnt[dma]
        self.q[eng].append(rec)
        return me

    def fence(self):
        deps = set()
        for k, w in self.lw.items():
            deps.add(w)
        for k, rs in self.rd.items():
            for r in rs:
                deps.add(r)
        if self.fence_op is not None:
            deps.add(self.fence_op)
        idx = len(self.q['dve'])
        me = ('dve', idx)
        deps.discard(me)
        self.q['dve'].append(dict(fn=lambda e: e.nop(), deps=deps, dma=None, inc=True, val=None))
        self.fence_op = me
        keep = lambda k: isinstance(k, tuple) and k[0] == 'ring'
        self.lw = {k: v for k, v in self.lw.items() if keep(k)}
        self.rd = {k: v for k, v in self.rd.items() if keep(k)}

    def finalize(self):
        q = self.q
        for e in ENGS:
            for op in q[e]:
                for (e2, i2) in op['deps']:
                    d = q[e2][i2]
                    if d['dma'] is None:
                        if e2 == 'pe' and e == 'pe' and op['dma'] is None:
                            continue
                        d['inc'] = True
        for e in ENGS:
            c = 0
            for op in q[e]:
                if op['dma'] is None and op['inc']:
                    c += 1
                    op['val'] = c
        for e in ENGS:
            waited = {}
            for op in q[e]:
                w = {}
                for (e2, i2) in op['deps']:
                    d = q[e2][i2]
                    if d['dma'] is None:
                        if e2 == 'pe' and e == 'pe' and op['dma'] is None:
                            continue
                        sk = ('eng', e2)
                    else:
                        sk = ('dma', d['dma'])
                    v = d['val']
                    if v > waited.get(sk, 0) and v > w.get(sk, 0):
                        w[sk] = v
                for sk, v in w.items():
                    waited[sk] = v
                op['waits'] = sorted(w.items(), key=str)

    def body(self, e, engsem, dmasem):
        def f(eng):
            for op in self.q[e]:
                for (sk, v) in op['waits']:
                    s = engsem[sk[1]] if sk[0] == 'eng' else dmasem[sk[1]]
                    eng.wait_ge(s, v)
                ins = op['fn'](eng)
                if op['dma'] is not None:
                    ins.then_inc(dmasem[op['dma']], 16)
                elif op['inc']:
                    ins.then_inc(engsem[e], 1)
            if e == 'sp':
                for k, v in self.dma_cnt.items():
                    eng.wait_ge(dmasem[k], v)
        return f


def make_consts():
    c = np.zeros((128, NCST), np.float32)
    i = np.arange(128)
    c[:, 0:128] = np.eye(128)
    c[:, 128:256] = (i[:, None] <= i[None, :])
    c[:, 256:384] = 1.0
    c[63, 384:512] = 1.0
    same = (i[:, None] // 64) == (i[None, :] // 64)
    tri = (i[:, None] <= i[None, :])
    mid = (i[:, None] % 64) <= 31
    c[:, 512:640] = same * (tri.astype(np.float32) - (mid & same).astype(np.float32))
    c[:, 640:768] = same * (1.0 - tri.astype(np.float32))
    c[:, 768] = (i // 64 == 0)
    c[:, 769] = (i // 64 == 1)
    c[:, 770] = (i // 64 == 0) & (i % 64 <= 31)
    c[:, 771] = (i // 64 == 1) & (i % 64 <= 31)
    c[:, 772:900] = same & tri
    c[:, 900] = 1.0
    c[:, 901] = (i % 64 <= 31)
    return c


def build(nseq, S, layers, dbg=None):
    NT = S // 128
    NTT = S // 512
    nc = bass.Bass("TRN2", target_bir_lowering=False)
    dt = nc.dram_tensor
    x_d = dt("x", [nseq, S, D], F32, kind="ExternalInput").ap()
    out_d = dt("out", [nseq, S, D], F32, kind="ExternalOutput").ap()
    win_d = dt("w_in", [4, D, NIN], F32, kind="ExternalInput").ap()
    wfb_d = dt("w_fb", [4, 512, D], F32, kind="ExternalInput").ap()
    whb_d = dt("w_hb", [4, 512, D], F32, kind="ExternalInput").ap()
    wmx_d = dt("w_mix", [4, D, D], F32, kind="ExternalInput").ap()
    w1_d = dt("w1", [4, NE, D, D], F32, kind="ExternalInput").ap()
    w3_d = dt("w3", [4, NE, D, D], F32, kind="ExternalInput").ap()
    w2_d = dt("w2", [4, NE, D, D], F32, kind="ExternalInput").ap()
    pf_d = dt("pf", [4, 128, NF], F32, kind="ExternalInput").ap()
    pfin_d = dt("pfin", [128, 16], F32, kind="ExternalInput").ap()
    pr_d = dt("pr", [4, 1, NR], F32, kind="ExternalInput").ap()
    lbl_d = dt("lbl", [1, 2048], F32, kind="ExternalInput").ap()
    rw_d = dt("rw", [128, 8 * 16], F32, kind="ExternalInput").ap()
    rb_d = dt("rb", [1, 16], F32, kind="ExternalInput").ap()
    cst_d = dt("cst", [128, NCST], F32, kind="ExternalInput").ap()
    dbg_d = None
    if dbg:
        dbg_d = dt("dbg", [len(dbg), D, S], F32, kind="ExternalOutput").ap()

    P = Prog()
    es = ExitStack()
    with es:
        SZ_X = 16 * S
        SZA = max(SZ_X, 32768)
        OFF_XH, OFF_XL, OFF_ACC = 0, SZ_X, SZ_X + SZA
        OFF_RING = OFF_ACC + 2 * SZA
        OFF_SM = OFF_RING + 4 * 8192
        TOTAL = OFF_SM + 16384
        arena = es.enter_context(nc.sbuf_tensor("arena", [128, TOTAL // 4], F32))

        def A(off, shape, dtp):
            assert off % 4 == 0
            nb = int(np.prod(shape[1:])) * (2 if dtp == BF16 else 4)
            assert nb % 4 == 0
            v = arena[:, off // 4:(off + nb) // 4]
            if dtp != F32:
                v = v.bitcast(dtp)
            if len(shape) == 3:
                v = v.rearrange("p (a b) -> p a b", a=shape[1])
            elif len(shape) == 4:
                v = v.rearrange("p (a b c) -> p a b c", a=shape[1], b=shape[2])
            return v

        XH = A(OFF_XH, [128, 8, S], BF16)
        XL = A(OFF_XL, [128, 8, S], BF16)
        ACC = A(OFF_ACC, [128, 8, S], F32)
        RING = [A(OFF_RING + i * 8192, [128, 8, 512], BF16) for i in range(4)]
        RING4 = [A(OFF_RING + i * 8192, [128, 4, 1024], BF16) for i in range(4)]
        o = OFF_SM
        def SM(shape, dtp):
            nonlocal o
            nb = int(np.prod(shape[1:])) * (2 if dtp == BF16 else 4)
            nb = (nb + 3) // 4 * 4
            v = A(o, shape, dtp)
            o += nb
            return v
        CST = SM([128, NCST], F32)
        IDB = SM([128, 128], BF16)
        MASKB = SM([128, 128], BF16)
        CMASKB = SM([128, 128], BF16)
        OM = SM([128, 512], F32)
        PF = SM([128, NF], F32)
        PFIN = SM([128, 16], F32)
        RW = SM([128, 8, 16], F32)
        RBT = SM([128, 16], F32)
        WFF = SM([128, 8, 8], BF16)
        BROW = SM([128, 512], F32)
        NGT = SM([128, 128], F32)
        BFFT = SM([128, 8], F32)
        LN_MEAN = SM([128, 512], F32)
        LN_RSTD = SM([128, 512], F32)
        LN_T = [SM([128, 512], F32), None]
        assert o <= TOTAL, (o, TOTAL)
        IDF = CST[:, 0:128]
        TI = CST[:, 128:256]
        ONES = CST[:, 256:384]
        SEL63 = CST[:, 384:512]
        A1 = CST[:, 512:640]
        A2 = CST[:, 640:768]
        CIND = CST[:, 768:772]
        DIND = CST[:, 900:902]
        R1 = OFF_ACC
        R2 = OFF_ACC + SZA
        FOXO = A(R1, [128, 4, S], BF16)
        HGO = A(R1 + SZA // 2, [128, 4, S], BF16)

        PS = [es.enter_context(nc.psum_tensor(f"ps{i}", [128, 512], F32)) for i in range(7)]
        PSB = es.enter_context(nc.psum_tensor("ps7", [128, 1024], BF16))
        engsem = {e: es.enter_context(nc.semaphore("s_" + e)) for e in ENGS}
        dkeys = ['ring0', 'ring1', 'ring2', 'ring3', 'x0', 'x1', 'pf', 'dcst', 'dpfin', 'drw', 'drbt', 'dbfft', 'wff', 'brow', 'ngt', 'lbl', 'out', 'dbg']
        dmasem = {k: es.enter_context(nc.semaphore("d_" + k)) for k in dkeys}
        block = es.enter_context(nc.Block())

        def psk(b):
            return ('ps', b)

        def mm(out, lhsT, rhs, start, stop, rd, wr):
            P.add('pe', lambda e: e.matmul(out, lhsT=lhsT, rhs=rhs, start=start, stop=stop), rd=rd, wr=wr)

        def tr(out, in_, ident, rd, wr):
            P.add('pe', lambda e: e.transpose(out=out, in_=in_, identity=ident), rd=rd, wr=wr)

        def act(out, in_, func, rd, wr, bias=None, scale=1.0):
            if bias is None:
                P.add('act', lambda e: e.activation(out=out, in_=in_, func=func, scale=scale), rd=rd, wr=wr)
            else:
                P.add('act', lambda e: e.activation(out=out, in_=in_, func=func, bias=bias, scale=scale), rd=rd, wr=wr)

        def tt(out, in0, in1, op, rd, wr, eng='dve'):
            P.add(eng, lambda e: e.tensor_tensor(out=out, in0=in0, in1=in1, op=op), rd=rd, wr=wr)

        def ts(out, in0, s1, op0, rd, wr, s2=None, op1=None, eng='dve'):
            if op1 is None:
                P.add(eng, lambda e: e.tensor_scalar(out=out, in0=in0, scalar1=s1, scalar2=None, op0=op0), rd=rd, wr=wr)
            else:
                P.add(eng, lambda e: e.tensor_scalar(out=out, in0=in0, scalar1=s1, scalar2=s2, op0=op0, op1=op1), rd=rd, wr=wr)

        def stt(out, in0, scalar, in1, op0, op1, rd, wr):
            P.add('dve', lambda e: e.scalar_tensor_tensor(out=out, in0=in0, scalar=scalar, in1=in1, op0=op0, op1=op1), rd=rd, wr=wr)

        def cp(out, in_, rd, wr, eng='dve'):
            if eng == 'act':
                P.add(eng, lambda e: e.activation(out=out, in_=in_, func=AF.Copy), rd=rd, wr=wr)
            else:
                P.add(eng, lambda e: e.tensor_copy(out=out, in_=in_), rd=rd, wr=wr)

        def memset(ap, val, wr, eng='dve'):
            P.add(eng, lambda e: e.memset(ap, val), wr=wr)

        def red(out, in_, op, rd, wr):
            P.add('dve', lambda e: e.tensor_reduce(out=out, in_=in_, op=op, axis=AX.X), rd=rd, wr=wr)

        def recip(out, in_, rd, wr):
            P.add('dve', lambda e: e.reciprocal(out=out, in_=in_), rd=rd, wr=wr)

        def dma(eng, out, in_, rd, wr, sem, nofence=False):
            return P.add(eng, lambda e: e.dma_start(out=out, in_=in_), rd=rd, wr=wr, dma=sem, nofence=nofence)

        ring_state = {'n': 0}

        def ringkeys(s):
            return [('ring', s, p) for p in range(4)]

        def slab(parts, four=False):
            s = ring_state['n'] % 4
            ring_state['n'] += 1
            view = RING4[s] if four else RING[s]
            ops = []
            for pi, (src, kc0, kcn, col0, ncols) in enumerate(parts):
                dst = view[:, kc0:kc0 + kcn, col0:col0 + ncols]
                ops.append(dma('pool', dst, src.rearrange("(c p) n -> p c n", p=128), rd=[], wr=[('ring', s, pi)], sem=f'ring{s}', nofence=True))
            for (e_, i_) in ops:
                P.q[e_][i_]['val'] = P.dma_cnt[f'ring{s}']
            return s

        dma('sp', CST, cst_d, [], ['cst'], 'dcst')
        dma('sp', PFIN, pfin_d, [], ['pfin'], 'dpfin')
        dma('sp', RW, rw_d.rearrange("p (c e) -> p c e", c=8), [], ['rw'], 'drw')
        dma('sp', RBT, rb_d.partition_broadcast(128), [], ['rbt'], 'drbt')
        cp(IDB, IDF, ['cst'], ['idb'])
        cp(MASKB, TI, ['cst'], ['maskb'])
        cp(CMASKB, CST[:, 772:900], ['cst'], ['cmaskb'])

        def xkeys(name, tti, cs=range(8)):
            return [(name, c, tti) for c in cs]

        dbg_i = {'n': 0}

        def dump_fm(name, fn_chunk, keys):
            if not dbg or name not in dbg:
                return
            k = dbg.index(name)
            for c in range(8):
                src = fn_chunk(c)
                if src is None:
                    continue
                tmp = LN_MEAN
                for t0 in range(0, S, 512):
                    cp(tmp, src[:, t0:t0 + 512], keys, ['lnmean'])
                    dma('sp', dbg_d[k, c * 128:(c + 1) * 128, t0:t0 + 512], tmp, ['lnmean'], ['dbgout'], 'dbg')

        def ln_tile(Yc, ykeys, tti, gcol, bcol, gb_key, final, seq):
            c0 = tti * 512
            tk = ['lnT0', 'lnT1']
            T = [LN_T[0], LN_RSTD]
            for c in range(8):
                mm(PS[6][:, :], ONES, Yc(c), c == 0, c == 7, ykeys(c) + ['cst'], [psk(6)])
            act(LN_MEAN, PS[6][:, :], AF.Identity, [psk(6)], ['lnmean'], scale=1.0 / D)
            for c in range(8):
                b = c % 2
                act(T[b], Yc(c), AF.Square, ykeys(c), [tk[b]])
                mm(PS[5][:, :], ONES, T[b], c == 0, c == 7, [tk[b], 'cst'], [psk(5)])
            tt(T[0], LN_MEAN, LN_MEAN, ALU.mult, ['lnmean'], [tk[0]])
            stt(LN_RSTD, PS[5][:, :], 1.0 / D, T[0], ALU.mult, ALU.subtract, [psk(5), tk[0]], [tk[1]])
            ts(LN_RSTD, LN_RSTD, LN_EPS, ALU.add, [tk[1]], [tk[1]])
            act(LN_RSTD, LN_RSTD, AF.Sqrt, [tk[1]], [tk[1]])
            recip(LN_RSTD, LN_RSTD, [tk[1]], [tk[1]])
            OUTT = A(OFF_XL, [128, 4, 1024], F32)
            for c in range(8):
                t = T[0]
                tt(t, Yc(c), LN_MEAN, ALU.subtract, ykeys(c) + ['lnmean'], [tk[0]])
                tt(t, t, LN_RSTD, ALU.mult, [tk[0], tk[1]], [tk[0]])
                act(t, t, AF.Identity, [tk[0], gb_key], [tk[0]], bias=bcol(c), scale=gcol(c))
                if not final:
                    cp(XH[:, c, c0:c0 + 512], t, [tk[0]], [('XH', c, tti)])
                    tt(XL[:, c, c0:c0 + 512], t, XH[:, c, c0:c0 + 512], ALU.subtract, [tk[0], ('XH', c, tti)], [('XL', c, tti)])
                else:
                    pb = 0 + (c % 2)
                    for q in range(4):
                        tr(PS[pb][:, q * 128:(q + 1) * 128], t[:, q * 128:(q + 1) * 128], IDF, [tk[0], 'cst'], [psk(pb)])
                    cp(OUTT[:, :, c * 128:(c + 1) * 128], PS[pb][:, :].rearrange("p (q f) -> p q f", q=4), [psk(pb)], ['outt'], eng='act' if False else 'dve')
            if final:
                dma('sp', out_d[seq, c0:c0 + 512, :].rearrange("(q p) d -> p q d", p=128), OUTT, ['outt'], ['outd'], 'out')

        import os
        STOP = os.environ.get('KSTOP', '')
        class _Stop(Exception):
            pass
        def stop_at(name):
            if STOP == name:
                raise _Stop()
        try:
          for seq in range(nseq):
              P.fence()
              XTOK = [A(R2 + i * 4096, [128, 1024], F32) for i in range(2)]
              Y0 = A(R1, [128, 8, 512], F32)
              for tti in range(NTT):
                  for q in range(4):
                      ti = tti * 4 + q
                      xb = ti % 2
                      dma('sp', XTOK[xb], x_d[seq, ti * 128:(ti + 1) * 128, :], [], [('xtok', xb)], f'x{xb}')
                      for hb in range(2):
                          for c4 in range(4):
                              c = hb * 4 + c4
                              tr(PS[hb][:, c4 * 128:(c4 + 1) * 128], XTOK[xb][:, c * 128:(c + 1) * 128], IDF, [('xtok', xb), 'cst'], [psk(hb)])
                          cp(Y0[:, hb * 4:(hb + 1) * 4, q * 128:(q + 1) * 128], PS[hb][:, :].rearrange("p (c t) -> p c t", c=4), [psk(hb)], [('y0', hb)],
                             eng='dve')
                  ln_tile(lambda c: Y0[:, c, :], lambda c: [('y0', c // 4)], tti, lambda c: PFIN[:, c:c + 1], lambda c: PFIN[:, 8 + c:9 + c], 'pfin', False, seq)
              if seq == 0:
                  dump_fm('ln_in', lambda c: XH[:, c, :], [k for t_ in range(NTT) for k in xkeys('XH', t_)])
              stop_at('p0')

              for li, l in enumerate(layers):
                  last = (li == len(layers) - 1)
                  P.fence()
                  dma('sp', PF, pf_d[l], [], ['pf'], 'pf')
                  BQK = lambda c: PF[:, c:c + 1]
                  BFV64 = PF[0:64, 68:76]
                  if l == 0:
                      memset(OM, 1.0, ['om'])
                  else:
                      LBT = A(R2, [128, 4, 512], F32)
                      LBM = A(R2 + 8192, [128, 512], F32)
                      LBS = A(R2 + 8192 + 2048, [128, 512], F32)
                      dma('sp', LBT, lbl_d.partition_broadcast(128), [], ['lbt'], 'lbl')
                      tt(LBM, LBT[:, 0, :], LBT[:, 1, :], ALU.max, ['lbt'], ['lbm'])
                      tt(LBM, LBM, LBT[:, 2, :], ALU.max, ['lbt', 'lbm'], ['lbm'])
                      tt(LBM, LBM, LBT[:, 3, :], ALU.max, ['lbt', 'lbm'], ['lbm'])
                      for r in range(4):
                          tt(LBT[:, r, :], LBT[:, r, :], LBM, ALU.subtract, ['lbt', 'lbm'], ['lbt'])
                      act(LBT, LBT, AF.Exp, ['lbt'], ['lbt'])
                      tt(LBS, LBT[:, 0, :], LBT[:, 1, :], ALU.add, ['lbt'], ['lbs'])
                      tt(LBS, LBS, LBT[:, 2, :], ALU.add, ['lbt', 'lbs'], ['lbs'])
                      tt(LBS, LBS, LBT[:, 3, :], ALU.add, ['lbt', 'lbs'], ['lbs'])
                      recip(LBS, LBS, ['lbs'], ['lbs'])
                      cp(LBM, LBT[:, 1, :], ['lbt'], ['lbm'])
                      for r in range(2, l + 1):
                          tt(LBM, LBM, LBT[:, r, :], ALU.add, ['lbt', 'lbm'], ['lbm'])
                      tt(LBM, LBM, LBS, ALU.mult, ['lbm', 'lbs'], ['lbm'])
                      ts(OM, LBM, -1.0, ALU.mult, ['lbm'], ['om'], s2=1.0, op1=ALU.add)
                  P.fence()

                  o2 = R2
                  QT = A(o2, [128, S], BF16); o2 += 2 * S
                  KT = A(o2, [128, S], BF16); o2 += 2 * S
                  VP = A(o2, [128, NT, 2, 66], BF16); o2 += NT * 2 * 66 * 2
                  CB = A(o2, [128, 2, NT, NT], F32); o2 += 2 * NT * NT * 4
                  SPT = A(o2, [128, NT * 8], F32); o2 += NT * 8 * 4
                  GTOK = A(o2, [128, NT, 8], F32); o2 += NT * 8 * 4
                  GREF = A(o2, [128, NT, 8], F32); o2 += NT * 8 * 4
                  OFFT = A(o2, [128, NT, 8], F32); o2 += NT * 8 * 4
                  TOTT = A(o2, [128, NT, 8], F32); o2 += NT * 8 * 4
                  PT = [A(o2 + i * 1024, [128, 512], BF16) for i in range(3)]; o2 += 3072
                  OTMP = A(o2, [128, 512], F32); o2 += 2048
                  RSB = A(o2, [128, 512], F32); o2 += 2048
                  RS = A(o2, [128, 512], F32); o2 += 2048
                  assert o2 <= R2 + SZA, (o2 - R2, SZA)

                  dma('pool', WFF, win_d[l][:, 1536:1544].rearrange("(c p) n -> p c n", p=128), [], ['wff'], 'wff')
                  dma('sp', BFFT, pr_d[l, :, 0:8].partition_broadcast(128), [], ['bfft'], 'dbfft')
                  allxh = [k for t_ in range(NTT) for k in xkeys('XH', t_)]
                  for i in range(NT):
                      mm(PS[6][:, i * 8:(i + 1) * 8], CST[0:1, 256:384], BFFT[0:1, :], True, False, ['cst', 'bfft'], [psk(6)])
                      for k in range(8):
                          mm(PS[6][:, i * 8:(i + 1) * 8], XH[:, k, i * 128:(i + 1) * 128], WFF[:, k, :], False, k == 7, [('XH', k, i // 4), 'wff'], [psk(6)])
                  act(SPT, PS[6][:, 0:NT * 8], AF.Exp, [psk(6)], ['spt'], scale=-1.0)
                  act(SPT, SPT, AF.Ln, ['spt'], ['spt'], bias=1.0, scale=1.0)
                  mm(PS[6][:, 0:NT * 8], TI, SPT, True, True, ['cst', 'spt'], [psk(6)])
                  mm(PS[5][:, 0:NT * 8], ONES, SPT, True, True, ['cst', 'spt'], [psk(5)])
                  cp(TOTT, PS[5][:, 0:NT * 8].rearrange("p (i h) -> p i h", h=8), [psk(5)], ['tott'])
                  memset(OFFT[:, 0, :], 0.0, ['offt'])
                  for i in range(1, NT):
                      tt(OFFT[:, i, :], OFFT[:, i - 1, :], TOTT[:, i - 1, :], ALU.add, ['offt', 'tott'], ['offt'])
                  tt(GTOK, PS[6][:, 0:NT * 8].rearrange("p (i h) -> p i h", h=8), OFFT, ALU.add, [psk(6), 'offt'], ['gtok'])
                  mm(PS[6][:, 0:NT * 8], SEL63, GTOK.rearrange("p i h -> p (i h)"), True, True, ['cst', 'gtok'], [psk(6)])
                  act(GREF, PS[6][:, 0:NT * 8].rearrange("p (i h) -> p i h", h=8), AF.Copy, [psk(6)], ['gref'])

                  stop_at('pF')
                  sti = 0
                  oi = 0
                  for j in range(4):
                      s = slab([(win_d[l][:, j * 128:(j + 1) * 128], 0, 8, 0, 128),
                                (win_d[l][:, 512 + j * 128:512 + (j + 1) * 128], 0, 8, 128, 128),
                                (win_d[l][:, 1024 + j * 128:1024 + (j + 1) * 128], 0, 8, 256, 128)])
                      W = RING[s]
                      rk = ringkeys(s)
                      for hp in range(2):
                          h = 2 * j + hp
                          tt(CB[:, hp, :, :], GTOK[:, :, h].unsqueeze(1).broadcast_to([128, NT, NT]), GREF[:, :, h].unsqueeze(2).broadcast_to([128, NT, NT]),
                             ALU.subtract, ['gtok', 'gref'], ['cb'])
                      for tti in range(NTT):
                          sl = slice(tti * 512, (tti + 1) * 512)
                          for wh, dst, nm in ((0, QT, 'qt'), (1, KT, 'kt')):
                              pb = sti % 4; sti += 1
                              for k in range(8):
                                  mm(PS[pb][:, :], W[:, k, wh * 128:(wh + 1) * 128], XH[:, k, sl], k == 0, k == 7, rk + [('XH', k, tti)], [psk(pb)])
                              act(dst[:, sl], PS[pb][:, :], AF.Identity, [psk(pb), 'pf'], [nm], bias=BQK(wh * 4 + j))
                      memset(VP[:, :, :, 64:65], 1.0, ['vp'])
                      for g in range(NTT):
                          pb = sti % 4; sti += 1
                          for q in range(4):
                              i = g * 4 + q
                              for k in range(8):
                                  mm(PS[pb][:, q * 128:(q + 1) * 128], XH[:, k, i * 128:(i + 1) * 128], W[:, k, 256:384], k == 0, k == 7, rk + [('XH', k, g)], [psk(pb)])
                          cp(VP[:, g * 4:(g + 1) * 4, :, 0:64], PS[pb][:, :].rearrange("p (q h e) -> p q h e", q=4, h=2), [psk(pb)], ['vp'])
                      steps = []
                      obank = {}
                      for hp in range(2):
                          for Q in range(NTT):
                              obank[(hp, Q)] = 4 + (oi % 2); oi += 1
                              for kt in range(Q * 4 + 4):
                                  steps.append((hp, Q, kt, Q * 4 + 4))
                      LOOK = 2

                      def emit_S(n):
                          nonlocal sti
                          hp, Q, kt, nk = steps[n]
                          hs = slice(hp * 64, (hp + 1) * 64)
                          q0 = Q * 512
                          qlo = max(kt * 128, q0)
                          cl = qlo - q0
                          pb = sti % 4; sti += 1
                          ptb = n % 3
                          mm(PS[pb][:, cl:512], KT[hs, kt * 128:(kt + 1) * 128], QT[hs, qlo:q0 + 512], True, True, ['kt', 'qt'], [psk(pb)])
                          for qt in range(qlo // 128, Q * 4 + 4):
                              c_ = qt * 128 - q0
                              act(PT[ptb][:, c_:c_ + 128], PS[pb][:, c_:c_ + 128], AF.Exp, [psk(pb), 'cb'], [('pt', ptb)], bias=CB[:, hp, qt, kt:kt + 1], scale=0.125)
                          if kt * 128 >= q0:
                              tt(PT[ptb][:, cl:cl + 128], PT[ptb][:, cl:cl + 128], MASKB, ALU.mult, [('pt', ptb), 'maskb'], [('pt', ptb)])

                      def emit_PV(n):
                          hp, Q, kt, nk = steps[n]
                          h = 2 * j + hp
                          hs = slice(hp * 64, (hp + 1) * 64)
                          q0 = Q * 512
                          cl = max(kt * 128, q0) - q0
                          ptb = n % 3
                          ob = obank[(hp, Q)]
                          mm(PS[ob][0:65, cl:512], VP[:, kt, hp, 0:65], PT[ptb][:, cl:512], kt == 0, kt == nk - 1, ['vp', ('pt', ptb)], [psk(ob)])
                          if kt == nk - 1:
                              recip(RS[64:65, :], PS[ob][64:65, :], [psk(ob)], ['rs'])
                              mm(PS[6][0:64, :], CST[64:65, 256:320], RS[64:65, :], True, True, ['cst', 'rs'], [psk(6)])
                              act(RSB[0:64, :], PS[6][0:64, :], AF.Copy, [psk(6)], ['rsb'])
                              tt(OTMP[0:64, :], PS[ob][0:64, :], RSB[0:64, :], ALU.mult, [psk(ob), 'rsb'], ['otmp'])
                              ts(FOXO[hs, j, q0:q0 + 512], OTMP[0:64, :], BFV64[:, h:h + 1], ALU.add, ['otmp', 'pf'], [('foxo', j)])

                      for n in range(min(LOOK, len(steps))):
                          emit_S(n)
                      for n in range(len(steps)):
                          if n + LOOK < len(steps):
                              emit_S(n + LOOK)
                          emit_PV(n)
                  dump_fm('foxo', lambda c: FOXO[:, c, :] if c < 4 else None, [('foxo', j) for j in range(4)])
                  stop_at('fox')
                  P.fence()

                  o2 = R2
                  def T2(shape, dtp, n=2):
                      nonlocal o2
                      r = []
                      for _ in range(n):
                          nb = int(np.prod(shape[1:])) * (2 if dtp == BF16 else 4)
                          r.append(A(o2, shape, dtp)); o2 += nb
                      return r
                  QF = T2([128, 128], F32); SIG = T2([128, 128], F32); KF = T2([128, 128], F32); LF = T2([128, 128], F32)
                  VB = T2([128, 128], BF16); GO = T2([128, 128], F32)
                  EU = T2([128, 128], F32); ENU = T2([128, 128], F32); EW = T2([128, 128], F32); DEC = T2([128, 4], F32)
                  QTL = T2([128, 128], BF16); KTL = T2([128, 128], BF16); KH = T2([128, 128], BF16)
                  QTT = T2([128, 128], BF16); KTT = T2([128, 128], BF16)
                  OF = T2([128, 128], F32); OSQ = T2([128, 128], F32); SS = T2([128, 2], F32); OG = T2([128, 128], BF16)
                  QX = [T2([128, 128], BF16) for _ in range(2)]; KX = [T2([128, 128], BF16) for _ in range(2)]; LX = [T2([128, 128], F32) for _ in range(2)]
                  QBD = [T2([128, 128], BF16) for _ in range(2)]; DECH = T2([128, 4], F32)
                  SMK = [T2([128, 128], BF16) for _ in range(2)]
                  STS = T2([128, 64], F32); SSB = T2([128, 64], BF16)
                  assert o2 <= R2 + SZA
                  for j in range(4):
                      s = slab([(win_d[l][:, 1544 + sl_ * 512 + j * 128:1544 + sl_ * 512 + (j + 1) * 128], 0, 8, sl_ * 128, 128) for sl_ in range(4)])
                      W = RING[s]
                      rk = ringkeys(s)
                      dma('sp', BROW, pr_d[l, :, 8 + j * 512:8 + (j + 1) * 512].partition_broadcast(128), [], ['brow'], 'brow')
                      dma('sp', NGT, pr_d[l, :, 2056 + j * 128:2056 + (j + 1) * 128].partition_broadcast(128), [], ['ngt'], 'ngt')
                      for hp in range(2):
                          memset(STS[hp], 0.0, [('ss', hp)])
                      OMJ = OM[:, j * 128:(j + 1) * 128]
                      def stageA(i):
                              b = i % 2
                              K = lambda n: (n, b)
                              tsl = slice(i * 128, (i + 1) * 128)
                              pa = 0 + b
                              mm(PS[pa][:, :], CST[0:1, 256:384], BROW[0:1, :], True, False, ['cst', 'brow'], [psk(pa)])
                              for k in range(8):
                                  mm(PS[pa][:, :], XH[:, k, tsl], W[:, k, :], False, k == 7, rk + [('XH', k, i // 4)], [psk(pa)])
                              act(QF[b], PS[pa][:, 0:128], AF.Silu, [psk(pa)], [K('qf')])
                              act(SIG[b], PS[pa][:, 128:256], AF.Sigmoid, [psk(pa)], [K('sig')])
                              act(VB[b], PS[pa][:, 256:384], AF.Copy, [psk(pa)], [K('vb')])
                              act(GO[b], PS[pa][:, 384:512], AF.Silu, [psk(pa)], [K('go')])
                              ts(SIG[b], SIG[b], -1.0, ALU.mult, [K('sig')], [K('sig')], s2=1.0, op1=ALU.add)
                              tt(KF[b], SIG[b], OMJ, ALU.mult, [K('sig'), 'om'], [K('kf')])
                              ts(LF[b], KF[b], -1.0, ALU.mult, [K('kf')], [K('lf')], s2=1.0, op1=ALU.add)
                              act(LF[b], LF[b], AF.Ln, [K('lf')], [K('lf')])
                              pbk = 2 + b
                              mm(PS[pbk][:, 0:128], A1, LF[b], True, True, ['cst', K('lf')], [psk(pbk)])
                              mm(PS[pbk][:, 128:256], A2, LF[b], True, True, ['cst', K('lf')], [psk(pbk)])
                              act(EU[b], PS[pbk][:, 0:128], AF.Exp, [psk(pbk)], [K('eu')])
                              act(ENU[b], PS[pbk][:, 0:128], AF.Exp, [psk(pbk)], [K('enu')], scale=-1.0)
                              act(EW[b], PS[pbk][:, 128:256], AF.Exp, [psk(pbk)], [K('ew')])
                              tt(QTL[b], QF[b], EU[b], ALU.mult, [K('qf'), K('eu')], [K('qtl')])
                              tt(KTL[b], KF[b], ENU[b], ALU.mult, [K('kf'), K('enu')], [K('ktl')])
                              tt(KH[b], KF[b], EW[b], ALU.mult, [K('kf'), K('ew')], [K('kh')])
                              tr(PSB[:, 0:128], QTL[b], IDB, [K('qtl'), 'idb'], [psk(7)])
                              tr(PSB[:, 128:256], KTL[b], IDB, [K('ktl'), 'idb'], [psk(7)])
                              cp(QTT[b], PSB[:, 0:128], [psk(7)], [K('qtt')])
                              cp(KTT[b], PSB[:, 128:256], [psk(7)], [K('ktt')])
                              stop_at('h3')
                              for hp in range(2):
                                  hs = slice(hp * 64, (hp + 1) * 64)
                                  for c2 in range(2):
                                      cc = slice(c2 * 64, (c2 + 1) * 64)
                                      ts(QX[b][hp][:, cc], QTL[b][:, hs], CST[:, 768 + c2:769 + c2], ALU.mult, [K('qtl'), 'cst'], [('qx', hp, b)])
                                      ts(KX[b][hp][:, cc], KH[b][:, hs], CST[:, 768 + c2:769 + c2], ALU.mult, [K('kh'), 'cst'], [('kx', hp, b)])
                                      ts(LX[b][hp][:, cc], LF[b][:, hs], CST[:, 768 + c2:769 + c2], ALU.mult, [K('lf'), 'cst'], [('lx', hp, b)])
                                  tr(PSB[:, 256 + hp * 128:384 + hp * 128], QX[b][hp], IDB, [('qx', hp, b), 'idb'], [psk(7)])
                                  cp(QBD[b][hp], PSB[:, 256 + hp * 128:384 + hp * 128], [psk(7)], [('qbd', hp, b)])
                                  mm(PS[4 + hp][:, 0:128], KTT[b][hs, :], QTT[b][hs, :], True, True, [K('ktt'), K('qtt')], [psk(4 + hp)])
                                  tt(SMK[b][hp], PS[4 + hp][:, 0:128], CMASKB, ALU.mult, [psk(4 + hp), 'cmaskb'], [('smk', hp, b)])
                              pbk = 2 + b
                              for hp in range(2):
                                  mm(PS[pbk][:, 384 + hp * 2:386 + hp * 2], LX[b][hp], DIND, True, True, [('lx', hp, b), 'cst'], [psk(pbk)])
                              act(DECH[b], PS[pbk][:, 384:388], AF.Exp, [psk(pbk)], [K('dech')])
                      def stageB(i):
                              b = i % 2
                              K = lambda n: (n, b)
                              tsl = slice(i * 128, (i + 1) * 128)
                              pbk = 2 + b
                              for hp in range(2):
                                  hs = slice(hp * 64, (hp + 1) * 64)
                                  mm(PS[6][:, hs], KX[b][hp], VB[b][:, hs], True, True, [('kx', hp, b), K('vb')], [psk(6)])
                              for hp in range(2):
                                  hs = slice(hp * 64, (hp + 1) * 64)
                                  stt(STS[hp][64:128, :], STS[hp][0:64, :], DECH[b][0:64, hp * 2:hp * 2 + 1], PS[6][0:64, hs], ALU.mult, ALU.add, [('ss', hp), K('dech'), psk(6)], [('ss', hp)])
                                  ts(SSB[hp], STS[hp], DECH[b][:, hp * 2 + 1:hp * 2 + 2], ALU.mult, [('ss', hp), K('dech')], [('ssb', hp)])
                                  stt(STS[hp][0:64, :], STS[hp][64:128, :], DECH[b][64:128, hp * 2:hp * 2 + 1], PS[6][64:128, hs], ALU.mult, ALU.add, [('ss', hp), K('dech'), psk(6)], [('ss', hp)])
                              for hp in range(2):
                                  hs = slice(hp * 64, (hp + 1) * 64)
                                  mm(PS[pbk][:, 256 + hp * 64:320 + hp * 64], SMK[b][hp], VB[b][:, hs], True, False, [('smk', hp, b), K('vb')], [psk(pbk)])
                                  mm(PS[pbk][:, 256 + hp * 64:320 + hp * 64], QBD[b][hp], SSB[hp], False, True, [('qbd', hp, b), ('ssb', hp)], [psk(pbk)])
                              act(OF[b], PS[pbk][:, 256:384], AF.Copy, [psk(pbk)], [K('of')])
                              tt(OSQ[b], OF[b], OF[b], ALU.mult, [K('of')], [K('osq')])
                              red(SS[b], OSQ[b].rearrange("p (h e) -> p h e", h=2), ALU.add, [K('osq')], [K('ss')])
                              act(SS[b], SS[b], AF.Sqrt, [K('ss')], [K('ss')], bias=RMS_EPS, scale=1.0 / 64)
                              recip(SS[b], SS[b], [K('ss')], [K('ss')])
                              tt(GO[b], GO[b], NGT, ALU.mult, [K('go'), 'ngt'], [K('go')])
                              tt(OF[b].rearrange("p (h e) -> p h e", h=2), OF[b].rearrange("p (h e) -> p h e", h=2), SS[b].unsqueeze(2).broadcast_to([128, 2, 64]),
                                 ALU.mult, [K('of'), K('ss')], [K('of')])
                              tt(OG[b], OF[b], GO[b], ALU.mult, [K('of'), K('go')], [K('og')])
                              tr(PSB[:, 512:640], OG[b], IDB, [K('og'), 'idb'], [psk(7)])
                              cp(HGO[:, j, tsl], PSB[:, 512:640], [psk(7)], [('hgo', j)])
                      stageA(0)
                      for i in range(NT):
                          if i + 1 < NT:
                              stageA(i + 1)
                          stageB(i)
                  dump_fm('hgo', lambda c: HGO[:, c, :] if c < 4 else None, [('hgo', j) for j in range(4)])
                  stop_at('hgrn')
                  P.fence()

                  MIXIN = A(R2, [128, 8, 512], BF16)
                  YM = A(R2 + 8192, [128, 8, 512], F32)
                  TG = [A(R2 + 8192 + 16384 + i * 1024, [128, 512], BF16) for i in range(2)]
                  TM = [A(R2 + 8192 + 16384 + 2048 + i * 2048, [128, 512], F32) for i in range(2)]
                  fk = [('foxo', j) for j in range(4)]
                  hk = [('hgo', j) for j in range(4)]
                  for tti in range(NTT):
                      sl = slice(tti * 512, (tti + 1) * 512)
                      for half in range(2):
                          s1 = slab([(win_d[l][:, 3592 + half * 512:3592 + (half + 1) * 512], 0, 8, 0, 512)])
                          s2 = slab([(win_d[l][:, 4616 + half * 512:4616 + (half + 1) * 512], 0, 8, 0, 512)])
                          s3 = slab([(wfb_d[l][:, half * 512:(half + 1) * 512], 0, 4, 0, 512), (whb_d[l][:, half * 512:(half + 1) * 512], 4, 4, 0, 512)])
                          for fc in range(4):
                              c = half * 4 + fc
                              fs = slice(fc * 128, (fc + 1) * 128)
                              for k in range(8):
                                  mm(PS[0][:, :], RING[s1][:, k, fs], XH[:, k, sl], k == 0, k == 7, ringkeys(s1) + [('XH', k, tti)], [psk(0)])
                              act(TG[0], PS[0][:, :], AF.Sigmoid, [psk(0), 'pf'], ['tg0'], bias=PF[:, 12 + c:13 + c])
                              for k in range(8):
                                  mm(PS[1][:, :], RING[s2][:, k, fs], XH[:, k, sl], k == 0, k == 7, ringkeys(s2) + [('XH', k, tti)], [psk(1)])
                              act(TG[1], PS[1][:, :], AF.Sigmoid, [psk(1), 'pf'], ['tg1'], bias=PF[:, 20 + c:21 + c])
                              for k in range(4):
                                  mm(PS[2][:, :], RING[s3][:, k, fs], FOXO[:, k, sl], k == 0, k == 3, ringkeys(s3) + fk, [psk(2)])
                              tt(TM[0], PS[2][:, :], TG[0], ALU.mult, [psk(2), 'tg0'], ['tm0'])
                              for k in range(4):
                                  mm(PS[3][:, :], RING[s3][:, 4 + k, fs], HGO[:, k, sl], k == 0, k == 3, ringkeys(s3) + hk, [psk(3)])
                              tt(TM[1], PS[3][:, :], TG[1], ALU.mult, [psk(3), 'tg1'], ['tm1'])
                              tt(MIXIN[:, c, :], TM[0], TM[1], ALU.add, ['tm0', 'tm1'], [('mixin', c)])
                      sm_ = [slab([(wmx_d[l][:, hf * 512:(hf + 1) * 512], 0, 8, 0, 512)]) for hf in range(2)]
                      for c in range(8):
                          s = sm_[c // 4]
                          fs = slice((c % 4) * 128, (c % 4 + 1) * 128)
                          pb = 4 + (c % 2)
                          for k in range(8):
                              mm(PS[pb][:, :], RING[s][:, k, fs], MIXIN[:, k, :], k == 0, k == 7, ringkeys(s) + [('mixin', k)], [psk(pb)])
                          act(YM[:, c, :], PS[pb][:, :], AF.Identity, [psk(pb), 'pf'], [('ym', c)], bias=PF[:, 28 + c:29 + c])
                          stt(YM[:, c, :], XH[:, c, sl], ALPHA, YM[:, c, :], ALU.mult, ALU.add, [('XH', c, tti), ('ym', c)], [('ym', c)])
                          stt(YM[:, c, :], XL[:, c, sl], ALPHA, YM[:, c, :], ALU.mult, ALU.add, [('XL', c, tti), ('ym', c)], [('ym', c)])
                      ln_tile(lambda c: YM[:, c, :], lambda c: [('ym', c)], tti, lambda c: PF[:, 36 + c:37 + c], lambda c: PF[:, 44 + c:45 + c], 'pf', False, seq)
                  if seq == 0:
                      dump_fm('ln1', lambda c: XH[:, c, :], [k for t_ in range(NTT) for k in xkeys('XH', t_)])
                  stop_at('merge')
                  P.fence()

                  for c in range(8):
                      ts(ACC[:, c, :], XH[:, c, :], ALPHA, ALU.mult, [('XH', c, t_) for t_ in range(NTT)], [('ACC', c, t_) for t_ in range(NTT)])
                      stt(ACC[:, c, :], XL[:, c, :], ALPHA, ACC[:, c, :], ALU.mult, ALU.add, [('XL', c, t_) for t_ in range(NTT)] + [('ACC', c, t_) for t_ in range(NTT)],
                          [('ACC', c, t_) for t_ in range(NTT)])
                  P.fence()
                  o3 = OFF_XL
                  def T3(shape, dtp):
                      nonlocal o3
                      nb = int(np.prod(shape[1:])) * (2 if dtp == BF16 else 4)
                      v = A(o3, shape, dtp); o3 += (nb + 3) // 4 * 4
                      return v
                  GT = T3([128, S], F32)
                  HT = [T3([128, 4, 512], BF16) for _ in range(2)]
                  SA = [T3([128, 512], BF16) for _ in range(2)]
                  TB = [T3([128, 512], BF16) for _ in range(2)]
                  GSB = T3([128, 512], F32)
                  RE = T3([128, 512], F32)
                  L_ = T3([128, NT, 16], F32); LM = T3([128, NT, 16], F32); IS1 = T3([128, NT, 16], F32); IS2 = T3([128, NT, 16], F32)
                  GG = T3([128, NT, 16], F32)
                  GMX = T3([128, NT, 4], F32); GMK = T3([128, NT, 4], F32); GT1 = T3([128, NT, 4], F32)
                  BMX = T3([128, NT], F32); M2 = T3([128, NT], F32); G1 = T3([128, NT], F32); G2 = T3([128, NT], F32)
                  assert o3 <= OFF_XL + SZA, (o3 - OFF_XL, SZA)
                  for i in range(NT):
                      for k in range(8):
                          mm(PS[6][:, i * 16:(i + 1) * 16], ACC[:, k, i * 128:(i + 1) * 128], RW[:, k, :], k == 0, k == 7, [('ACC', k, i // 4), 'rw'], [psk(6)])
                  stt(L_, PS[6][:, 0:NT * 16].rearrange("p (i e) -> p i e", e=16), 1.0 / ALPHA, RBT.unsqueeze(1).broadcast_to([128, NT, 16]), ALU.mult, ALU.add,
                      [psk(6), 'rbt'], ['L'])
                  red(GMX.rearrange("p i g -> p (i g)"), L_.rearrange("p i (g e) -> p (i g) e", g=4), ALU.max, ['L'], ['gmx'])
                  red(BMX, GMX, ALU.max, ['gmx'], ['bmx'])
                  tt(GMK, GMX, BMX.unsqueeze(2).broadcast_to([128, NT, 4]), ALU.is_equal, ['gmx', 'bmx'], ['gmk'])
                  ts(GT1, GMK, BIG, ALU.mult, ['gmk'], ['gt1'], s2=-BIG, op1=ALU.add)
                  L4 = lambda t_: t_.rearrange("p i (g e) -> p (i g) e", g=4)
                  G4 = lambda t_: t_.rearrange("p i g -> p (i g)").unsqueeze(2).broadcast_to([128, NT * 4, 4])
                  tt(L4(LM), L4(L_), G4(GMK), ALU.mult, ['L', 'gmk'], ['lm'])
                  tt(L4(LM), L4(LM), G4(GT1), ALU.add, ['lm', 'gt1'], ['lm'])
                  tt(IS1, LM, BMX.unsqueeze(2).broadcast_to([128, NT, 16]), ALU.is_equal, ['lm', 'bmx'], ['is1'])
                  stt(LM, IS1, -BIG, LM, ALU.mult, ALU.add, ['is1', 'lm'], ['lm'])
                  red(M2, LM, ALU.max, ['lm'], ['m2'])
                  tt(IS2, LM, M2.unsqueeze(2).broadcast_to([128, NT, 16]), ALU.is_equal, ['lm', 'm2'], ['is2'])
                  tt(G1, BMX, M2, ALU.subtract, ['bmx', 'm2'], ['g1'])
                  act(G1, G1, AF.Sigmoid, ['g1'], ['g1'])
                  ts(G2, G1, -1.0, ALU.mult, ['g1'], ['g2'], s2=1.0, op1=ALU.add)
                  tt(IS1, IS1, G1.unsqueeze(2).broadcast_to([128, NT, 16]), ALU.mult, ['is1', 'g1'], ['is1'])
                  tt(IS2, IS2, G2.unsqueeze(2).broadcast_to([128, NT, 16]), ALU.mult, ['is2', 'g2'], ['is2'])
                  tt(GG, IS1, IS2, ALU.add, ['is1', 'is2'], ['gg'])
                  for g in range(NTT):
                      for q in range(4):
                          i = g * 4 + q
                          tr(PS[6][0:16, q * 128:(q + 1) * 128], GG[:, i, :], IDF, ['gg', 'cst'], [psk(6)])
                      act(GT[0:16, g * 512:(g + 1) * 512], PS[6][0:16, :], AF.Copy, [psk(6)], ['gt'])
                  its = [(e, half, tti) for e in range(NE) for half in range(2) for tti in range(NTT)]
                  slabs_ = {}

                  def get_slabs(e, half):
                      if (e, half) not in slabs_:
                          sa_ = slab([(w1_d[l, e][:, half * 512:(half + 1) * 512], 0, 8, 0, 512)])
                          sb_ = slab([(w3_d[l, e][:, half * 512:(half + 1) * 512], 0, 8, 0, 512)])
                          sc_ = slab([(w2_d[l, e][half * 512:(half + 1) * 512, :], 0, 4, 0, 1024)], four=True)
                          slabs_[(e, half)] = (sa_, sb_, sc_)
                      return slabs_[(e, half)]

                  def stage_ab(n):
                      e, half, tti = its[n]
                      sa_, sb_, sc_ = get_slabs(e, half)
                      sl = slice(tti * 512, (tti + 1) * 512)
                      hb = n % 2
                      ts(RE[0:16, :], GT[0:16, sl], IDF[0:16, e:e + 1], ALU.mult, ['gt', 'cst'], ['re'])
                      mm(PS[6][:, :], ONES[0:16, :], RE[0:16, :], True, True, ['cst', 're'], [psk(6)])
                      act(GSB, PS[6][:, :], AF.Copy, [psk(6)], ['gsb'])
                      for hc in range(4):
                          fs = slice(hc * 128, (hc + 1) * 128)
                          b2 = hc % 2
                          for k in range(8):
                              mm(PS[b2][:, :], RING[sa_][:, k, fs], XH[:, k, sl], k == 0, k == 7, ringkeys(sa_) + [('XH', k, tti)], [psk(b2)])
                          act(SA[b2], PS[b2][:, :], AF.Silu, [psk(b2)], [('sa', b2)])
                          for k in range(8):
                              mm(PS[2 + b2][:, :], RING[sb_][:, k, fs], XH[:, k, sl], k == 0, k == 7, ringkeys(sb_) + [('XH', k, tti)], [psk(2 + b2)])
                          tt(TB[b2], PS[2 + b2][:, :], GSB, ALU.mult, [psk(2 + b2), 'gsb'], [('tb', b2)])
                          tt(HT[hb][:, hc, :], SA[b2], TB[b2], ALU.mult, [('sa', b2), ('tb', b2)], [('ht', hb, hc)])

                  def stage_w2(n):
                      e, half, tti = its[n]
                      sa_, sb_, sc_ = get_slabs(e, half)
                      sl = slice(tti * 512, (tti + 1) * 512)
                      hb = n % 2
                      for fc in range(8):
                          pb = 4 + (fc % 2)
                          for hc in range(4):
                              mm(PS[pb][:, :], RING4[sc_][:, hc, fc * 128:(fc + 1) * 128], HT[hb][:, hc, :], hc == 0, hc == 3, ringkeys(sc_) + [('ht', hb, hc)], [psk(pb)])
                          tt(ACC[:, fc, sl], PS[pb][:, :], ACC[:, fc, sl], ALU.add, [psk(pb), ('ACC', fc, tti)], [('ACC', fc, tti)])

                  stage_ab(0)
                  for n in range(len(its)):
                      if n + 1 < len(its):
                          stage_ab(n + 1)
                      stage_w2(n)
                  P.fence()
                  for tti in range(NTT):
                      sl = slice(tti * 512, (tti + 1) * 512)
                      ln_tile(lambda c: ACC[:, c, sl], lambda c: [('ACC', c, tti)], tti, lambda c: PF[:, 52 + c:53 + c], lambda c: PF[:, 60 + c:61 + c], 'pf', last, seq)
                  if seq == 0 and not last:
                      dump_fm('ln2', lambda c: XH[:, c, :], [k for t_ in range(NTT) for k in xkeys('XH', t_)])


        except _Stop:
            pass
        P.finalize()
        block.tensor(P.body('pe', engsem, dmasem))
        block.scalar(P.body('act', engsem, dmasem))
        block.vector(P.body('dve', engsem, dmasem))
        block.gpsimd(P.body('pool', engsem, dmasem))
        block.sync(P.body('sp', engsem, dmasem))
    return nc, P


def prep_inputs(inp):
    f = lambda a: np.ascontiguousarray(np.asarray(a, dtype=np.float32))
    b_in = f(inp['b_in'])
    L = b_in.shape[0]
    fm = lambda v: v.reshape(-1, 128).T
    pf = np.zeros((L, 128, NF), np.float32)
    pr = np.zeros((L, 1, NR), np.float32)
    for l in range(L):
        pf[l, :, 0:8] = fm(b_in[l, 0:1024])
        pf[l, :, 8:12] = fm(b_in[l, 1024:1536])
        pf[l, :, 12:28] = fm(b_in[l, 3592:5640])
        pf[l, :, 28:36] = fm(f(inp['b_mix_out'])[l])
        pf[l, :, 36:44] = fm(f(inp['ln1_g'])[l])
        pf[l, :, 44:52] = fm(f(inp['ln1_b'])[l])
        pf[l, :, 52:60] = fm(f(inp['ln2_g'])[l])
        pf[l, :, 60:68] = fm(f(inp['ln2_b'])[l])
        pf[l, 0:64, 68:76] = b_in[l, 1024:1536].reshape(8, 64).T
        pr[l, 0, 0:8] = b_in[l, 1536:1544]
        for j in range(4):
            for s in range(4):
                pr[l, 0, 8 + j * 512 + s * 128:8 + j * 512 + (s + 1) * 128] = b_in[l, 1544 + s * 512 + j * 128:1544 + s * 512 + (j + 1) * 128]
        pr[l, 0, 2056:2568] = f(inp['hgrn_norm_g'])[l]
    pfin = np.concatenate([fm(f(inp['ln_in_g'])), fm(f(inp['ln_in_b']))], axis=1)
    rw = f(inp['router_w']).reshape(8, 128, 16).transpose(1, 0, 2).reshape(128, 128)
    shared = {
        'w_in': f(inp['w_in']), 'w_fb': f(inp['w_fox_branch']), 'w_hb': f(inp['w_hgrn_branch']), 'w_mix': f(inp['w_mix_out']),
        'w1': f(inp['expert_w1']), 'w3': f(inp['expert_w3']), 'w2': f(inp['expert_w2']),
        'pf': pf, 'pfin': np.ascontiguousarray(pfin), 'pr': pr, 'lbl': f(inp['hgrn_lb_logits']).reshape(1, -1),
        'rw': np.ascontiguousarray(rw), 'rb': f(inp['router_b']).reshape(1, 16), 'cst': make_consts(),
    }
    return shared


_CACHE = {}


def kernel(**inputs):
    x = np.ascontiguousarray(np.asarray(inputs['x'], dtype=np.float32))
    B, S, _ = x.shape
    ncores = 8
    nseq = B // ncores
    depth = np.asarray(inputs['w_in']).shape[0]
    key = (nseq, S, depth)
    if key not in _CACHE:
        _CACHE[key] = build(nseq, S, list(range(depth)))[0]
    nc = _CACHE[key]
    shared = prep_inputs(inputs)
    in_maps = []
    for c in range(ncores):
        m = dict(shared)
        m['x'] = np.ascontiguousarray(x[c * nseq:(c + 1) * nseq])
        in_maps.append(m)
    res = run_bass_kernel_spmd(nc, in_maps, core_ids=list(range(ncores)))
    return np.concatenate([r['out'] for r in res.results], axis=0).astype(np.float32)
```

```python
import numpy as np
from contextlib import ExitStack
import concourse.bass as bass
import concourse.mybir as mybir
from concourse.bass_utils import run_bass_kernel_spmd

F32 = mybir.dt.float32
BF16 = mybir.dt.bfloat16
AF = mybir.ActivationFunctionType
ALU = mybir.AluOpType
AX = mybir.AxisListType

D = 1024
NIN = 5640
NE = 16
ALPHA = float(8 ** 0.25)
LN_EPS = 1e-5
RMS_EPS = 1e-6
BIG = 1.0e4
ENGS = ('pe', 'act', 'dve', 'pool', 'sp')
NF = 76
NR = 8 + 2048 + 512
NCST = 1030


class Prog:
    def __init__(self):
        self.q = {e: [] for e in ENGS}
        self.lw = {}
        self.rd = {}
        self.dma_cnt = {}
        self.fence_op = None

    def add(self, eng, fn, rd=(), wr=(), dma=None, nofence=False):
        idx = len(self.q[eng])
        me = (eng, idx)
        deps = set()
        for k in rd:
            w = self.lw.get(k)
            if w is not None:
                deps.add(w)
        for k in wr:
            w = self.lw.get(k)
            if w is not None:
                deps.add(w)
            for r in self.rd.get(k, ()):
                deps.add(r)
        if self.fence_op is not None and not nofence:
            deps.add(self.fence_op)
        deps.discard(me)
        for k in rd:
            self.rd.setdefault(k, []).append(me)
        for k in wr:
            self.lw[k] = me
            self.rd[k] = []
        rec = dict(fn=fn, deps=deps, dma=dma, inc=False, val=None)
        if dma is not None:
            self.dma_cnt[dma] = self.dma_cnt.get(dma, 0) + 16
            rec['val'] = self.dma_cnt[dma]
        self.q[eng].append(rec)
        return me

    def fence(self):
        deps = set()
        for k, w in self.lw.items():
            deps.add(w)
        for k, rs in self.rd.items():
            for r in rs:
                deps.add(r)
        if self.fence_op is not None:
            deps.add(self.fence_op)
        idx = len(self.q['dve'])
        me = ('dve', idx)
        deps.discard(me)
        self.q['dve'].append(dict(fn=lambda e: e.nop(), deps=deps, dma=None, inc=True, val=None))
        self.fence_op = me
        keep = lambda k: isinstance(k, tuple) and k[0] == 'ring'
        self.lw = {k: v for k, v in self.lw.items() if keep(k)}
        self.rd = {k: v for k, v in self.rd.items() if keep(k)}

    def finalize(self):
        q = self.q
        for e in ENGS:
            for op in q[e]:
                for (e2, i2) in op['deps']:
                    d = q[e2][i2]
                    if d['dma'] is None:
                        if e2 == 'pe' and e == 'pe' and op['dma'] is None:
                            continue
                        d['inc'] = True
        for e in ENGS:
            c = 0
            for op in q[e]:
                if op['dma'] is None and op['inc']:
                    c += 1
                    op['val'] = c
        for e in ENGS:
            waited = {}
            for op in q[e]:
                w = {}
                for (e2, i2) in op['deps']:
                    d = q[e2][i2]
                    if d['dma'] is None:
                        if e2 == 'pe' and e == 'pe' and op['dma'] is None:
                            continue
                        sk = ('eng', e2)
                    else:
                        sk = ('dma', d['dma'])
                    v = d['val']
                    if v > waited.get(sk, 0) and v > w.get(sk, 0):
                        w[sk] = v
                for sk, v in w.items():
                    waited[sk] = v
                op['waits'] = sorted(w.items(), key=str)

    def body(self, e, engsem, dmasem):
        def f(eng):
            for op in self.q[e]:
                for (sk, v) in op['waits']:
                    s = engsem[sk[1]] if sk[0] == 'eng' else dmasem[sk[1]]
                    eng.wait_ge(s, v)
                ins = op['fn'](eng)
                if op['dma'] is not None:
                    ins.then_inc(dmasem[op['dma']], 16)
                elif op['inc']:
                    ins.then_inc(engsem[e], 1)
            if e == 'sp':
                for k, v in self.dma_cnt.items():
                    eng.wait_ge(dmasem[k], v)
        return f


def make_consts():
    c = np.zeros((128, NCST), np.float32)
    i = np.arange(128)
    c[:, 0:128] = np.eye(128)
    c[:, 128:256] = (i[:, None] <= i[None, :])
    c[:, 256:384] = 1.0
    c[63, 384:512] = 1.0
    same = (i[:, None] // 64) == (i[None, :] // 64)
    tri = (i[:, None] <= i[None, :])
    mid = (i[:, None] % 64) <= 31
    c[:, 512:640] = same * (tri.astype(np.float32) - (mid & same).astype(np.float32))
    c[:, 640:768] = same * (1.0 - tri.astype(np.float32))
    c[:, 768] = (i // 64 == 0)
    c[:, 769] = (i // 64 == 1)
    c[:, 770] = (i // 64 == 0) & (i % 64 <= 31)
    c[:, 771] = (i // 64 == 1) & (i % 64 <= 31)
    c[:, 772:900] = same & tri
    c[:, 900] = 1.0
    c[:, 901] = (i % 64 <= 31)
    c[0, 902:1030] = 1.0
    c[32, 902:1030] = 1.0
    return c


def build(nseq, S, layers, dbg=None):
    NT = S // 128
    NTT = S // 512
    nc = bass.Bass("TRN2", target_bir_lowering=False)
    dt = nc.dram_tensor
    x_d = dt("x", [nseq, S, D], F32, kind="ExternalInput").ap()
    out_d = dt("out", [nseq, S, D], F32, kind="ExternalOutput").ap()
    win_d = dt("w_in", [4, D, NIN], F32, kind="ExternalInput").ap()
    wfb_d = dt("w_fb", [4, 512, D], F32, kind="ExternalInput").ap()
    whb_d = dt("w_hb", [4, 512, D], F32, kind="ExternalInput").ap()
    wmx_d = dt("w_mix", [4, D, D], F32, kind="ExternalInput").ap()
    w1_d = dt("w1", [4, NE, D, D], F32, kind="ExternalInput").ap()
    w3_d = dt("w3", [4, NE, D, D], F32, kind="ExternalInput").ap()
    w2_d = dt("w2", [4, NE, D, D], F32, kind="ExternalInput").ap()
    pf_d = dt("pf", [4, 128, NF], F32, kind="ExternalInput").ap()
    pfin_d = dt("pfin", [128, 16], F32, kind="ExternalInput").ap()
    pr_d = dt("pr", [4, 1, NR], F32, kind="ExternalInput").ap()
    lbl_d = dt("lbl", [1, 2048], F32, kind="ExternalInput").ap()
    rw_d = dt("rw", [128, 8 * 16], F32, kind="ExternalInput").ap()
    rb_d = dt("rb", [1, 16], F32, kind="ExternalInput").ap()
    cst_d = dt("cst", [128, NCST], F32, kind="ExternalInput").ap()
    dbg_d = None
    if dbg:
        dbg_d = dt("dbg", [len(dbg), D, S], F32, kind="ExternalOutput").ap()

    P = Prog()
    es = ExitStack()
    with es:
        SZ_X = 16 * S
        SZA = max(SZ_X, 32768)
        OFF_XH, OFF_XL, OFF_ACC = 0, SZ_X, SZ_X + SZA
        OFF_RING = OFF_ACC + 2 * SZA
        OFF_SM = OFF_RING + 4 * 8192
        TOTAL = OFF_SM + 16384
        arena = es.enter_context(nc.sbuf_tensor("arena", [128, TOTAL // 4], F32))

        def A(off, shape, dtp):
            assert off % 4 == 0
            nb = int(np.prod(shape[1:])) * (2 if dtp == BF16 else 4)
            assert nb % 4 == 0
            v = arena[:, off // 4:(off + nb) // 4]
            if dtp != F32:
                v = v.bitcast(dtp)
            if len(shape) == 3:
                v = v.rearrange("p (a b) -> p a b", a=shape[1])
            elif len(shape) == 4:
                v = v.rearrange("p (a b c) -> p a b c", a=shape[1], b=shape[2])
            return v

        XH = A(OFF_XH, [128, 8, S], BF16)
        XL = A(OFF_XL, [128, 8, S], BF16)
        ACC = A(OFF_ACC, [128, 8, S], F32)
        RING = [A(OFF_RING + i * 8192, [128, 8, 512], BF16) for i in range(4)]
        RING4 = [A(OFF_RING + i * 8192, [128, 4, 1024], BF16) for i in range(4)]
        o = OFF_SM
        def SM(shape, dtp):
            nonlocal o
            nb = int(np.prod(shape[1:])) * (2 if dtp == BF16 else 4)
            nb = (nb + 3) // 4 * 4
            v = A(o, shape, dtp)
            o += nb
            return v
        CST = SM([128, NCST], F32)
        IDB = SM([128, 128], BF16)
        MASKB = SM([128, 128], BF16)
        CMASKB = SM([128, 128], BF16)
        OM = SM([128, 512], F32)
        PF = SM([128, NF], F32)
        PFIN = SM([128, 16], F32)
        RW = SM([128, 8, 16], F32)
        RBT = SM([128, 16], F32)
        WFF = SM([128, 8, 8], BF16)
        ONESELB = SM([128, 128], BF16)
        BFFT = SM([128, 8], F32)
        LN_MEAN = SM([128, 512], F32)
        LN_RSTD = SM([128, 512], F32)
        LN_T = [SM([128, 512], F32), None]
        assert o <= TOTAL, (o, TOTAL)
        IDF = CST[:, 0:128]
        TI = CST[:, 128:256]
        ONES = CST[:, 256:384]
        SEL63 = CST[:, 384:512]
        A1 = CST[:, 512:640]
        A2 = CST[:, 640:768]
        CIND = CST[:, 768:772]
        DIND = CST[:, 900:902]
        R1 = OFF_ACC
        R2 = OFF_ACC + SZA
        FOXO = A(R1, [128, 4, S], BF16)
        HGO = A(R1 + SZA // 2, [128, 4, S], BF16)

        PS = [es.enter_context(nc.psum_tensor(f"ps{i}", [128, 512], F32)) for i in range(7)]
        PSB = es.enter_context(nc.psum_tensor("ps7", [128, 1024], BF16))
        engsem = {e: es.enter_context(nc.semaphore("s_" + e)) for e in ENGS}
        dkeys = ['ring0', 'ring1', 'ring2', 'ring3', 'x0', 'x1', 'pf', 'dcst', 'dpfin', 'drw', 'drbt', 'dbfft', 'wff', 'brow0', 'brow1', 'ngt0', 'ngt1', 'lbl', 'out', 'dbg']
        dmasem = {k: es.enter_context(nc.semaphore("d_" + k)) for k in dkeys}
        block = es.enter_context(nc.Block())

        def psk(b):
            return ('ps', b)

        def mm(out, lhsT, rhs, start, stop, rd, wr):
            P.add('pe', lambda e: e.matmul(out, lhsT=lhsT, rhs=rhs, start=start, stop=stop), rd=rd, wr=wr)

        def tr(out, in_, ident, rd, wr):
            P.add('pe', lambda e: e.transpose(out=out, in_=in_, identity=ident), rd=rd, wr=wr)

        def act(out, in_, func, rd, wr, bias=None, scale=1.0):
            if bias is None:
                P.add('act', lambda e: e.activation(out=out, in_=in_, func=func, scale=scale), rd=rd, wr=wr)
            else:
                P.add('act', lambda e: e.activation(out=out, in_=in_, func=func, bias=bias, scale=scale), rd=rd, wr=wr)

        def tt(out, in0, in1, op, rd, wr, eng='dve'):
            P.add(eng, lambda e: e.tensor_tensor(out=out, in0=in0, in1=in1, op=op), rd=rd, wr=wr)

        def ts(out, in0, s1, op0, rd, wr, s2=None, op1=None, eng='dve'):
            if op1 is None:
                P.add(eng, lambda e: e.tensor_scalar(out=out, in0=in0, scalar1=s1, scalar2=None, op0=op0), rd=rd, wr=wr)
            else:
                P.add(eng, lambda e: e.tensor_scalar(out=out, in0=in0, scalar1=s1, scalar2=s2, op0=op0, op1=op1), rd=rd, wr=wr)

        def stt(out, in0, scalar, in1, op0, op1, rd, wr):
            P.add('dve', lambda e: e.scalar_tensor_tensor(out=out, in0=in0, scalar=scalar, in1=in1, op0=op0, op1=op1), rd=rd, wr=wr)

        def cp(out, in_, rd, wr, eng='dve'):
            if eng == 'act':
                P.add(eng, lambda e: e.activation(out=out, in_=in_, func=AF.Copy), rd=rd, wr=wr)
            else:
                P.add(eng, lambda e: e.tensor_copy(out=out, in_=in_), rd=rd, wr=wr)

        def memset(ap, val, wr, eng='dve'):
            P.add(eng, lambda e: e.memset(ap, val), wr=wr)

        def red(out, in_, op, rd, wr):
            P.add('dve', lambda e: e.tensor_reduce(out=out, in_=in_, op=op, axis=AX.X), rd=rd, wr=wr)

        def recip(out, in_, rd, wr):
            P.add('dve', lambda e: e.reciprocal(out=out, in_=in_), rd=rd, wr=wr)

        def dma(eng, out, in_, rd, wr, sem, nofence=False):
            return P.add(eng, lambda e: e.dma_start(out=out, in_=in_), rd=rd, wr=wr, dma=sem, nofence=nofence)

        ring_state = {'n': 0}

        def ringkeys(s):
            return [('ring', s, p) for p in range(4)]

        def slab(parts, four=False):
            s = ring_state['n'] % 4
            ring_state['n'] += 1
            view = RING4[s] if four else RING[s]
            ops = []
            for pi, (src, kc0, kcn, col0, ncols) in enumerate(parts):
                dst = view[:, kc0:kc0 + kcn, col0:col0 + ncols]
                ops.append(dma('pool', dst, src.rearrange("(c p) n -> p c n", p=128), rd=[], wr=[('ring', s, pi)], sem=f'ring{s}', nofence=True))
            for (e_, i_) in ops:
                P.q[e_][i_]['val'] = P.dma_cnt[f'ring{s}']
            return s

        dma('sp', CST, cst_d, [], ['cst'], 'dcst')
        dma('sp', PFIN, pfin_d, [], ['pfin'], 'dpfin')
        dma('sp', RW, rw_d.rearrange("p (c e) -> p c e", c=8), [], ['rw'], 'drw')
        dma('sp', RBT, rb_d.partition_broadcast(128), [], ['rbt'], 'drbt')
        cp(IDB, IDF, ['cst'], ['idb'])
        cp(MASKB, TI, ['cst'], ['maskb'])
        cp(CMASKB, CST[:, 772:900], ['cst'], ['cmaskb'])
        cp(ONESELB, CST[:, 902:1030], ['cst'], ['oneselb'])

        def xkeys(name, tti, cs=range(8)):
            return [(name, c, tti) for c in cs]

        dbg_i = {'n': 0}

        def dump_fm(name, fn_chunk, keys):
            if not dbg or name not in dbg:
                return
            k = dbg.index(name)
            for c in range(8):
                src = fn_chunk(c)
                if src is None:
                    continue
                tmp = LN_MEAN
                for t0 in range(0, S, 512):
                    cp(tmp, src[:, t0:t0 + 512], keys, ['lnmean'])
                    dma('sp', dbg_d[k, c * 128:(c + 1) * 128, t0:t0 + 512], tmp, ['lnmean'], ['dbgout'], 'dbg')

        def ln_tile(Yc, ykeys, tti, gcol, bcol, gb_key, final, seq):
            c0 = tti * 512
            tk = ['lnT0', 'lnT1']
            T = [LN_T[0], LN_RSTD]
            for c in range(8):
                mm(PS[6][:, :], ONES, Yc(c), c == 0, c == 7, ykeys(c) + ['cst'], [psk(6)])
            act(LN_MEAN, PS[6][:, :], AF.Identity, [psk(6)], ['lnmean'], scale=1.0 / D)
            for c in range(8):
                b = c % 2
                act(T[b], Yc(c), AF.Square, ykeys(c), [tk[b]])
                mm(PS[5][:, :], ONES, T[b], c == 0, c == 7, [tk[b], 'cst'], [psk(5)])
            tt(T[0], LN_MEAN, LN_MEAN, ALU.mult, ['lnmean'], [tk[0]])
            stt(LN_RSTD, PS[5][:, :], 1.0 / D, T[0], ALU.mult, ALU.subtract, [psk(5), tk[0]], [tk[1]])
            ts(LN_RSTD, LN_RSTD, LN_EPS, ALU.add, [tk[1]], [tk[1]])
            act(LN_RSTD, LN_RSTD, AF.Sqrt, [tk[1]], [tk[1]])
            recip(LN_RSTD, LN_RSTD, [tk[1]], [tk[1]])
            OUTT = A(OFF_XL, [128, 4, 1024], F32)
            for c in range(8):
                t = T[0]
                tt(t, Yc(c), LN_MEAN, ALU.subtract, ykeys(c) + ['lnmean'], [tk[0]])
                tt(t, t, LN_RSTD, ALU.mult, [tk[0], tk[1]], [tk[0]])
                act(t, t, AF.Identity, [tk[0], gb_key], [tk[0]], bias=bcol(c), scale=gcol(c))
                if not final:
                    cp(XH[:, c, c0:c0 + 512], t, [tk[0]], [('XH', c, tti)])
                    tt(XL[:, c, c0:c0 + 512], t, XH[:, c, c0:c0 + 512], ALU.subtract, [tk[0], ('XH', c, tti)], [('XL', c, tti)])
                else:
                    pb = 0 + (c % 2)
                    for q in range(4):
                        tr(PS[pb][:, q * 128:(q + 1) * 128], t[:, q * 128:(q + 1) * 128], IDF, [tk[0], 'cst'], [psk(pb)])
                    cp(OUTT[:, :, c * 128:(c + 1) * 128], PS[pb][:, :].rearrange("p (q f) -> p q f", q=4), [psk(pb)], ['outt'], eng='act' if False else 'dve')
            if final:
                dma('sp', out_d[seq, c0:c0 + 512, :].rearrange("(q p) d -> p q d", p=128), OUTT, ['outt'], ['outd'], 'out')

        import os
        STOP = os.environ.get('KSTOP', '')
        class _Stop(Exception):
            pass
        def stop_at(name):
            if STOP == name:
                raise _Stop()
        try:
          for seq in range(nseq):
              P.fence()
              XTOK = [A(R2 + i * 4096, [128, 1024], F32) for i in range(2)]
              Y0 = A(R1, [128, 8, 512], F32)
              for tti in range(NTT):
                  for q in range(4):
                      ti = tti * 4 + q
                      xb = ti % 2
                      dma('sp', XTOK[xb], x_d[seq, ti * 128:(ti + 1) * 128, :], [], [('xtok', xb)], f'x{xb}')
                      for hb in range(2):
                          for c4 in range(4):
                              c = hb * 4 + c4
                              tr(PS[hb][:, c4 * 128:(c4 + 1) * 128], XTOK[xb][:, c * 128:(c + 1) * 128], IDF, [('xtok', xb), 'cst'], [psk(hb)])
                          cp(Y0[:, hb * 4:(hb + 1) * 4, q * 128:(q + 1) * 128], PS[hb][:, :].rearrange("p (c t) -> p c t", c=4), [psk(hb)], [('y0', hb)],
                             eng='dve')
                  ln_tile(lambda c: Y0[:, c, :], lambda c: [('y0', c // 4)], tti, lambda c: PFIN[:, c:c + 1], lambda c: PFIN[:, 8 + c:9 + c], 'pfin', False, seq)
              if seq == 0:
                  dump_fm('ln_in', lambda c: XH[:, c, :], [k for t_ in range(NTT) for k in xkeys('XH', t_)])
              stop_at('p0')

              for li, l in enumerate(layers):
                  last = (li == len(layers) - 1)
                  P.fence()
                  dma('sp', PF, pf_d[l], [], ['pf'], 'pf')
                  BQK = lambda c: PF[:, c:c + 1]
                  BFV64 = PF[0:64, 68:76]
                  if l == 0:
                      memset(OM, 1.0, ['om'])
                  else:
                      LBT = A(R2, [128, 4, 512], F32)
                      LBM = A(R2 + 8192, [128, 512], F32)
                      LBS = A(R2 + 8192 + 2048, [128, 512], F32)
                      dma('sp', LBT, lbl_d.partition_broadcast(128), [], ['lbt'], 'lbl')
                      tt(LBM, LBT[:, 0, :], LBT[:, 1, :], ALU.max, ['lbt'], ['lbm'])
                      tt(LBM, LBM, LBT[:, 2, :], ALU.max, ['lbt', 'lbm'], ['lbm'])
                      tt(LBM, LBM, LBT[:, 3, :], ALU.max, ['lbt', 'lbm'], ['lbm'])
                      for r in range(4):
                          tt(LBT[:, r, :], LBT[:, r, :], LBM, ALU.subtract, ['lbt', 'lbm'], ['lbt'])
                      act(LBT, LBT, AF.Exp, ['lbt'], ['lbt'])
                      tt(LBS, LBT[:, 0, :], LBT[:, 1, :], ALU.add, ['lbt'], ['lbs'])
                      tt(LBS, LBS, LBT[:, 2, :], ALU.add, ['lbt', 'lbs'], ['lbs'])
                      tt(LBS, LBS, LBT[:, 3, :], ALU.add, ['lbt', 'lbs'], ['lbs'])
                      recip(LBS, LBS, ['lbs'], ['lbs'])
                      cp(LBM, LBT[:, 1, :], ['lbt'], ['lbm'])
                      for r in range(2, l + 1):
                          tt(LBM, LBM, LBT[:, r, :], ALU.add, ['lbt', 'lbm'], ['lbm'])
                      tt(LBM, LBM, LBS, ALU.mult, ['lbm', 'lbs'], ['lbm'])
                      ts(OM, LBM, -1.0, ALU.mult, ['lbm'], ['om'], s2=1.0, op1=ALU.add)
                  P.fence()

                  o2 = R2
                  QT = A(o2, [128, S], BF16); o2 += 2 * S
                  KT = A(o2, [128, S], BF16); o2 += 2 * S
                  VP = A(o2, [128, NT, 2, 66], BF16); o2 += NT * 2 * 66 * 2
                  CB = A(o2, [128, 2, NT, NT], F32); o2 += 2 * NT * NT * 4
                  SPT = A(o2, [128, NT * 8], F32); o2 += NT * 8 * 4
                  GTOK = A(o2, [128, NT, 8], F32); o2 += NT * 8 * 4
                  GREF = A(o2, [128, NT, 8], F32); o2 += NT * 8 * 4
                  OFFT = A(o2, [128, NT, 8], F32); o2 += NT * 8 * 4
                  TOTT = A(o2, [128, NT, 8], F32); o2 += NT * 8 * 4
                  PT = [A(o2 + i * 1024, [128, 512], BF16) for i in range(3)]; o2 += 3072
                  OTMP = A(o2, [128, 512], F32); o2 += 2048
                  RSB = A(o2, [128, 512], F32); o2 += 2048
                  RS = A(o2, [128, 512], F32); o2 += 2048
                  assert o2 <= R2 + SZA, (o2 - R2, SZA)

                  dma('pool', WFF, win_d[l][:, 1536:1544].rearrange("(c p) n -> p c n", p=128), [], ['wff'], 'wff')
                  dma('sp', BFFT, pr_d[l, :, 0:8].partition_broadcast(128), [], ['bfft'], 'dbfft')
                  allxh = [k for t_ in range(NTT) for k in xkeys('XH', t_)]
                  for i in range(NT):
                      mm(PS[6][:, i * 8:(i + 1) * 8], CST[0:1, 256:384], BFFT[0:1, :], True, False, ['cst', 'bfft'], [psk(6)])
                      for k in range(8):
                          mm(PS[6][:, i * 8:(i + 1) * 8], XH[:, k, i * 128:(i + 1) * 128], WFF[:, k, :], False, k == 7, [('XH', k, i // 4), 'wff'], [psk(6)])
                  act(SPT, PS[6][:, 0:NT * 8], AF.Exp, [psk(6)], ['spt'], scale=-1.0)
                  act(SPT, SPT, AF.Ln, ['spt'], ['spt'], bias=1.0, scale=1.0)
                  mm(PS[6][:, 0:NT * 8], TI, SPT, True, True, ['cst', 'spt'], [psk(6)])
                  mm(PS[5][:, 0:NT * 8], ONES, SPT, True, True, ['cst', 'spt'], [psk(5)])
                  cp(TOTT, PS[5][:, 0:NT * 8].rearrange("p (i h) -> p i h", h=8), [psk(5)], ['tott'])
                  memset(OFFT[:, 0, :], 0.0, ['offt'])
                  for i in range(1, NT):
                      tt(OFFT[:, i, :], OFFT[:, i - 1, :], TOTT[:, i - 1, :], ALU.add, ['offt', 'tott'], ['offt'])
                  tt(GTOK, PS[6][:, 0:NT * 8].rearrange("p (i h) -> p i h", h=8), OFFT, ALU.add, [psk(6), 'offt'], ['gtok'])
                  mm(PS[6][:, 0:NT * 8], SEL63, GTOK.rearrange("p i h -> p (i h)"), True, True, ['cst', 'gtok'], [psk(6)])
                  act(GREF, PS[6][:, 0:NT * 8].rearrange("p (i h) -> p i h", h=8), AF.Copy, [psk(6)], ['gref'])

                  stop_at('pF')
                  sti = 0
                  oi = 0
                  for j in range(4):
                      s = slab([(win_d[l][:, j * 128:(j + 1) * 128], 0, 8, 0, 128),
                                (win_d[l][:, 512 + j * 128:512 + (j + 1) * 128], 0, 8, 128, 128),
                                (win_d[l][:, 1024 + j * 128:1024 + (j + 1) * 128], 0, 8, 256, 128)])
                      W = RING[s]
                      rk = ringkeys(s)
                      for hp in range(2):
                          h = 2 * j + hp
                          tt(CB[:, hp, :, :], GTOK[:, :, h].unsqueeze(1).broadcast_to([128, NT, NT]), GREF[:, :, h].unsqueeze(2).broadcast_to([128, NT, NT]),
                             ALU.subtract, ['gtok', 'gref'], ['cb'])
                      for tti in range(NTT):
                          sl = slice(tti * 512, (tti + 1) * 512)
                          for wh, dst, nm in ((0, QT, 'qt'), (1, KT, 'kt')):
                              pb = sti % 4; sti += 1
                              for k in range(8):
                                  mm(PS[pb][:, :], W[:, k, wh * 128:(wh + 1) * 128], XH[:, k, sl], k == 0, k == 7, rk + [('XH', k, tti)], [psk(pb)])
                              act(dst[:, sl], PS[pb][:, :], AF.Identity, [psk(pb), 'pf'], [nm], bias=BQK(wh * 4 + j))
                      memset(VP[:, :, :, 64:65], 1.0, ['vp'])
                      for g in range(NTT):
                          pb = sti % 4; sti += 1
                          for q in range(4):
                              i = g * 4 + q
                              for k in range(8):
                                  mm(PS[pb][:, q * 128:(q + 1) * 128], XH[:, k, i * 128:(i + 1) * 128], W[:, k, 256:384], k == 0, k == 7, rk + [('XH', k, g)], [psk(pb)])
                          cp(VP[:, g * 4:(g + 1) * 4, :, 0:64], PS[pb][:, :].rearrange("p (q h e) -> p q h e", q=4, h=2), [psk(pb)], ['vp'])
                      steps = []
                      obank = {}
                      for hp in range(2):
                          for Q in range(NTT):
                              obank[(hp, Q)] = 4 + (oi % 2); oi += 1
                              for kt in range(Q * 4 + 4):
                                  steps.append((hp, Q, kt, Q * 4 + 4))
                      LOOK = 2

                      def emit_S(n):
                          nonlocal sti
                          hp, Q, kt, nk = steps[n]
                          hs = slice(hp * 64, (hp + 1) * 64)
                          q0 = Q * 512
                          qlo = max(kt * 128, q0)
                          cl = qlo - q0
                          pb = sti % 4; sti += 1
                          ptb = n % 3
                          mm(PS[pb][:, cl:512], KT[hs, kt * 128:(kt + 1) * 128], QT[hs, qlo:q0 + 512], True, True, ['kt', 'qt'], [psk(pb)])
                          for qt in range(qlo // 128, Q * 4 + 4):
                              c_ = qt * 128 - q0
                              act(PT[ptb][:, c_:c_ + 128], PS[pb][:, c_:c_ + 128], AF.Exp, [psk(pb), 'cb'], [('pt', ptb)], bias=CB[:, hp, qt, kt:kt + 1], scale=0.125)
                          if kt * 128 >= q0:
                              tt(PT[ptb][:, cl:cl + 128], PT[ptb][:, cl:cl + 128], MASKB, ALU.mult, [('pt', ptb), 'maskb'], [('pt', ptb)])

                      def emit_PV(n):
                          hp, Q, kt, nk = steps[n]
                          h = 2 * j + hp
                          hs = slice(hp * 64, (hp + 1) * 64)
                          q0 = Q * 512
                          cl = max(kt * 128, q0) - q0
                          ptb = n % 3
                          ob = obank[(hp, Q)]
                          mm(PS[ob][0:65, cl:512], VP[:, kt, hp, 0:65], PT[ptb][:, cl:512], kt == 0, kt == nk - 1, ['vp', ('pt', ptb)], [psk(ob)])
                          if kt == nk - 1:
                              recip(RS[64:65, :], PS[ob][64:65, :], [psk(ob)], ['rs'])
                              mm(PS[6][0:64, :], CST[64:65, 256:320], RS[64:65, :], True, True, ['cst', 'rs'], [psk(6)])
                              act(RSB[0:64, :], PS[6][0:64, :], AF.Copy, [psk(6)], ['rsb'])
                              tt(OTMP[0:64, :], PS[ob][0:64, :], RSB[0:64, :], ALU.mult, [psk(ob), 'rsb'], ['otmp'])
                              ts(FOXO[hs, j, q0:q0 + 512], OTMP[0:64, :], BFV64[:, h:h + 1], ALU.add, ['otmp', 'pf'], [('foxo', j)])

                      for n in range(min(LOOK, len(steps))):
                          emit_S(n)
                      for n in range(len(steps)):
                          if n + LOOK < len(steps):
                              emit_S(n + LOOK)
                          emit_PV(n)
                  dump_fm('foxo', lambda c: FOXO[:, c, :] if c < 4 else None, [('foxo', j) for j in range(4)])
                  stop_at('fox')
                  P.fence()

                  o2 = R2
                  def T2(shape, dtp, n=2):
                      nonlocal o2
                      r = []
                      for _ in range(n):
                          nb = int(np.prod(shape[1:])) * (2 if dtp == BF16 else 4)
                          r.append(A(o2, shape, dtp)); o2 += (nb + 3) // 4 * 4
                      return r
                  QF = T2([128, 128], F32); SIG = T2([128, 128], F32); KF = T2([128, 128], F32); LF = T2([128, 128], F32)
                  VB = T2([128, 128], BF16); GO = T2([128, 128], F32)
                  EU = T2([128, 128], F32); ENU = T2([128, 128], F32); EW = T2([128, 128], F32)
                  QTL = T2([128, 128], BF16); KTL = T2([128, 128], BF16); KH = T2([128, 128], BF16)
                  QTT = T2([128, 128], BF16); KTT = T2([128, 128], BF16)
                  OF = T2([128, 128], F32); OSQ = T2([128, 128], F32); SS = T2([128, 2], F32); OG = T2([128, 128], BF16)
                  QXA = T2([128, 2, 2, 64], BF16); KXA = T2([128, 2, 2, 64], BF16); LXA = T2([128, 2, 2, 64], F32)
                  QBD = [T2([128, 128], BF16) for _ in range(2)]; DECH = T2([128, 4], F32)
                  SMK = [T2([128, 128], BF16) for _ in range(2)]
                  STS = [T2([128, 64], F32) for _ in range(2)]; SSB = [T2([128, 64], BF16) for _ in range(2)]
                  BROW2 = T2([128, 512], F32); BB = T2([128, 512], BF16); NGT2 = T2([128, 128], F32)
                  assert o2 <= R2 + SZA, (o2 - R2, SZA)

                  def pair_gen(j, p):
                      s = slab([(win_d[l][:, 1544 + sl_ * 512 + j * 128:1544 + sl_ * 512 + (j + 1) * 128], 0, 8, sl_ * 128, 128) for sl_ in range(4)])
                      W = RING[s]
                      rk = ringkeys(s)
                      K = lambda n: (n, p)
                      dma('sp', BROW2[p], pr_d[l, :, 8 + j * 512:8 + (j + 1) * 512].partition_broadcast(128), [], [K('brow')], f'brow{p}')
                      dma('sp', NGT2[p], pr_d[l, :, 2056 + j * 128:2056 + (j + 1) * 128].partition_broadcast(128), [], [K('ngt')], f'ngt{p}')
                      cp(BB[p], BROW2[p], [K('brow')], [K('bb')])
                      tt(BB[p][32:64, :], BROW2[p][32:64, :], BB[p][32:64, :], ALU.subtract, [K('brow'), K('bb')], [K('bb')])
                      for hp in range(2):
                          memset(STS[p][hp], 0.0, [('ss', hp, p)])
                      OMJ = OM[:, j * 128:(j + 1) * 128]
                      pa = 0 + p
                      pbk = 2 + p
                      yield
                      for i in range(NT):
                          tsl = slice(i * 128, (i + 1) * 128)
                          mm(PS[pa][:, :], ONESELB[0:33, :], BB[p][0:33, :], True, False, ['oneselb', K('bb')], [psk(pa)])
                          for k in range(8):
                              mm(PS[pa][:, :], XH[:, k, tsl], W[:, k, :], False, k == 7, rk + [('XH', k, i // 4)], [psk(pa)])
                          yield
                          act(QF[p], PS[pa][:, 0:128], AF.Silu, [psk(pa)], [K('qf')])
                          act(GO[p], PS[pa][:, 384:512], AF.Silu, [psk(pa)], [K('go')])
                          yield
                          act(SIG[p], PS[pa][:, 128:256], AF.Sigmoid, [psk(pa)], [K('sig')])
                          act(VB[p], PS[pa][:, 256:384], AF.Copy, [psk(pa)], [K('vb')])
                          yield
                          ts(SIG[p], SIG[p], -1.0, ALU.mult, [K('sig')], [K('sig')], s2=1.0, op1=ALU.add)
                          tt(KF[p], SIG[p], OMJ, ALU.mult, [K('sig'), 'om'], [K('kf')])
                          ts(LF[p], KF[p], -1.0, ALU.mult, [K('kf')], [K('lf')], s2=1.0, op1=ALU.add)
                          yield
                          act(LF[p], LF[p], AF.Ln, [K('lf')], [K('lf')])
                          yield
                          mm(PS[pbk][:, 0:128], A1, LF[p], True, True, ['cst', K('lf')], [psk(pbk)])
                          mm(PS[pbk][:, 128:256], A2, LF[p], True, True, ['cst', K('lf')], [psk(pbk)])
                          tt(LXA[p], LF[p].rearrange("q (h d) -> q h d", h=2).unsqueeze(2).broadcast_to([128, 2, 2, 64]),
                             CST[:, 768:770].unsqueeze(1).unsqueeze(3).broadcast_to([128, 2, 2, 64]), ALU.mult, [K('lf'), 'cst'], [K('lxa')])
                          yield
                          act(EU[p], PS[pbk][:, 0:128], AF.Exp, [psk(pbk)], [K('eu')])
                          act(ENU[p], PS[pbk][:, 0:128], AF.Exp, [psk(pbk)], [K('enu')], scale=-1.0)
                          act(EW[p], PS[pbk][:, 128:256], AF.Exp, [psk(pbk)], [K('ew')])
                          yield
                          for hp in range(2):
                              mm(PS[pbk][:, 384 + hp * 2:386 + hp * 2], LXA[p][:, hp].rearrange("q c d -> q (c d)"), DIND, True, True, [K('lxa'), 'cst'], [psk(pbk)])
                          tt(QTL[p], QF[p], EU[p], ALU.mult, [K('qf'), K('eu')], [K('qtl')])
                          tt(KTL[p], KF[p], ENU[p], ALU.mult, [K('kf'), K('enu')], [K('ktl')])
                          tt(KH[p], KF[p], EW[p], ALU.mult, [K('kf'), K('ew')], [K('kh')])
                          yield
                          act(DECH[p], PS[pbk][:, 384:388], AF.Exp, [psk(pbk)], [K('dech')])
                          tr(PSB[:, 0:128], QTL[p], IDB, [K('qtl'), 'idb'], [psk(7)])
                          tr(PSB[:, 128:256], KTL[p], IDB, [K('ktl'), 'idb'], [psk(7)])
                          cp(QTT[p], PSB[:, 0:128], [psk(7)], [K('qtt')])
                          cp(KTT[p], PSB[:, 128:256], [psk(7)], [K('ktt')])
                          yield
                          tt(QXA[p], QTL[p].rearrange("q (h d) -> q h d", h=2).unsqueeze(2).broadcast_to([128, 2, 2, 64]),
                             CST[:, 768:770].unsqueeze(1).unsqueeze(3).broadcast_to([128, 2, 2, 64]), ALU.mult, [K('qtl'), 'cst'], [K('qxa')])
                          tt(KXA[p], KH[p].rearrange("q (h d) -> q h d", h=2).unsqueeze(2).broadcast_to([128, 2, 2, 64]),
                             CST[:, 768:770].unsqueeze(1).unsqueeze(3).broadcast_to([128, 2, 2, 64]), ALU.mult, [K('kh'), 'cst'], [K('kxa')])
                          yield
                          for hp in range(2):
                              hs = slice(hp * 64, (hp + 1) * 64)
                              tr(PSB[:, 256 + hp * 128:384 + hp * 128], QXA[p][:, hp].rearrange("q c d -> q (c d)"), IDB, [K('qxa'), 'idb'], [psk(7)])
                              cp(QBD[p][hp], PSB[:, 256 + hp * 128:384 + hp * 128], [psk(7)], [('qbd', hp, p)])
                              mm(PS[4 + hp][:, 0:128], KTT[p][hs, :], QTT[p][hs, :], True, True, [K('ktt'), K('qtt')], [psk(4 + hp)])
                              tt(SMK[p][hp], PS[4 + hp][:, 0:128], CMASKB, ALU.mult, [psk(4 + hp), 'cmaskb'], [('smk', hp, p)])
                              yield
                          for hp in range(2):
                              hs = slice(hp * 64, (hp + 1) * 64)
                              mm(PS[6][:, hs], KXA[p][:, hp].rearrange("q c d -> q (c d)"), VB[p][:, hs], True, True, [K('kxa'), K('vb')], [psk(6)])
                          for hp in range(2):
                              hs = slice(hp * 64, (hp + 1) * 64)
                              stt(STS[p][hp][64:128, :], STS[p][hp][0:64, :], DECH[p][0:64, hp * 2:hp * 2 + 1], PS[6][0:64, hs], ALU.mult, ALU.add,
                                  [('ss', hp, p), K('dech'), psk(6)], [('ss', hp, p)])
                              ts(SSB[p][hp], STS[p][hp], DECH[p][:, hp * 2 + 1:hp * 2 + 2], ALU.mult, [('ss', hp, p), K('dech')], [('ssb', hp, p)])
                              stt(STS[p][hp][0:64, :], STS[p][hp][64:128, :], DECH[p][64:128, hp * 2:hp * 2 + 1], PS[6][64:128, hs], ALU.mult, ALU.add,
                                  [('ss', hp, p), K('dech'), psk(6)], [('ss', hp, p)])
                          yield
                          for hp in range(2):
                              hs = slice(hp * 64, (hp + 1) * 64)
                              mm(PS[pbk][:, 256 + hp * 64:320 + hp * 64], SMK[p][hp], VB[p][:, hs], True, False, [('smk', hp, p), K('vb')], [psk(pbk)])
                              mm(PS[pbk][:, 256 + hp * 64:320 + hp * 64], QBD[p][hp], SSB[p][hp], False, True, [('qbd', hp, p), ('ssb', hp, p)], [psk(pbk)])
                          yield
                          act(OF[p], PS[pbk][:, 256:384], AF.Copy, [psk(pbk)], [K('of')])
                          tt(GO[p], GO[p], NGT2[p], ALU.mult, [K('go'), K('ngt')], [K('go')])
                          yield
                          tt(OSQ[p], OF[p], OF[p], ALU.mult, [K('of')], [K('osq')])
                          red(SS[p], OSQ[p].rearrange("q (h e) -> q h e", h=2), ALU.add, [K('osq')], [K('ss')])
                          yield
                          act(SS[p], SS[p], AF.Sqrt, [K('ss')], [K('ss')], bias=RMS_EPS, scale=1.0 / 64)
                          yield
                          recip(SS[p], SS[p], [K('ss')], [K('ss')])
                          tt(OF[p].rearrange("q (h e) -> q h e", h=2), OF[p].rearrange("q (h e) -> q h e", h=2), SS[p].unsqueeze(2).broadcast_to([128, 2, 64]),
                             ALU.mult, [K('of'), K('ss')], [K('of')])
                          tt(OG[p], OF[p], GO[p], ALU.mult, [K('of'), K('go')], [K('og')])
                          yield
                          tr(PSB[:, 512:640], OG[p], IDB, [K('og'), 'idb'], [psk(7)])
                          cp(HGO[:, j, tsl], PSB[:, 512:640], [psk(7)], [('hgo', j)])
                          yield

                  for jj in (0, 2):
                      alive = [pair_gen(jj, 0), pair_gen(jj + 1, 1)]
                      while alive:
                          for g_ in list(alive):
                              try:
                                  next(g_)
                              except StopIteration:
                                  alive.remove(g_)
                  dump_fm('hgo', lambda c: HGO[:, c, :] if c < 4 else None, [('hgo', j) for j in range(4)])
                  stop_at('hgrn')
                  P.fence()

                  MIXIN = A(R2, [128, 8, 512], BF16)
                  YM = A(R2 + 8192, [128, 8, 512], F32)
                  TG = [A(R2 + 8192 + 16384 + i * 1024, [128, 512], BF16) for i in range(2)]
                  TM = [A(R2 + 8192 + 16384 + 2048 + i * 2048, [128, 512], F32) for i in range(2)]
                  fk = [('foxo', j) for j in range(4)]
                  hk = [('hgo', j) for j in range(4)]
                  for tti in range(NTT):
                      sl = slice(tti * 512, (tti + 1) * 512)
                      for half in range(2):
                          s1 = slab([(win_d[l][:, 3592 + half * 512:3592 + (half + 1) * 512], 0, 8, 0, 512)])
                          s2 = slab([(win_d[l][:, 4616 + half * 512:4616 + (half + 1) * 512], 0, 8, 0, 512)])
                          s3 = slab([(wfb_d[l][:, half * 512:(half + 1) * 512], 0, 4, 0, 512), (whb_d[l][:, half * 512:(half + 1) * 512], 4, 4, 0, 512)])
                          for fc in range(4):
                              c = half * 4 + fc
                              fs = slice(fc * 128, (fc + 1) * 128)
                              for k in range(8):
                                  mm(PS[0][:, :], RING[s1][:, k, fs], XH[:, k, sl], k == 0, k == 7, ringkeys(s1) + [('XH', k, tti)], [psk(0)])
                              act(TG[0], PS[0][:, :], AF.Sigmoid, [psk(0), 'pf'], ['tg0'], bias=PF[:, 12 + c:13 + c])
                              for k in range(8):
                                  mm(PS[1][:, :], RING[s2][:, k, fs], XH[:, k, sl], k == 0, k == 7, ringkeys(s2) + [('XH', k, tti)], [psk(1)])
                              act(TG[1], PS[1][:, :], AF.Sigmoid, [psk(1), 'pf'], ['tg1'], bias=PF[:, 20 + c:21 + c])
                              for k in range(4):
                                  mm(PS[2][:, :], RING[s3][:, k, fs], FOXO[:, k, sl], k == 0, k == 3, ringkeys(s3) + fk, [psk(2)])
                              tt(TM[0], PS[2][:, :], TG[0], ALU.mult, [psk(2), 'tg0'], ['tm0'])
                              for k in range(4):
                                  mm(PS[3][:, :], RING[s3][:, 4 + k, fs], HGO[:, k, sl], k == 0, k == 3, ringkeys(s3) + hk, [psk(3)])
                              tt(TM[1], PS[3][:, :], TG[1], ALU.mult, [psk(3), 'tg1'], ['tm1'])
                              tt(MIXIN[:, c, :], TM[0], TM[1], ALU.add, ['tm0', 'tm1'], [('mixin', c)])
                      sm_ = [slab([(wmx_d[l][:, hf * 512:(hf + 1) * 512], 0, 8, 0, 512)]) for hf in range(2)]
                      for c in range(8):
                          s = sm_[c // 4]
                          fs = slice((c % 4) * 128, (c % 4 + 1) * 128)
                          pb = 4 + (c % 2)
                          for k in range(8):
                              mm(PS[pb][:, :], RING[s][:, k, fs], MIXIN[:, k, :], k == 0, k == 7, ringkeys(s) + [('mixin', k)], [psk(pb)])
                          act(YM[:, c, :], PS[pb][:, :], AF.Identity, [psk(pb), 'pf'], [('ym', c)], bias=PF[:, 28 + c:29 + c])
                          stt(YM[:, c, :], XH[:, c, sl], ALPHA, YM[:, c, :], ALU.mult, ALU.add, [('XH', c, tti), ('ym', c)], [('ym', c)])
                          stt(YM[:, c, :], XL[:, c, sl], ALPHA, YM[:, c, :], ALU.mult, ALU.add, [('XL', c, tti), ('ym', c)], [('ym', c)])
                      ln_tile(lambda c: YM[:, c, :], lambda c: [('ym', c)], tti, lambda c: PF[:, 36 + c:37 + c], lambda c: PF[:, 44 + c:45 + c], 'pf', False, seq)
                  if seq == 0:
                      dump_fm('ln1', lambda c: XH[:, c, :], [k for t_ in range(NTT) for k in xkeys('XH', t_)])
                  stop_at('merge')
                  P.fence()

                  for c in range(8):
                      ts(ACC[:, c, :], XH[:, c, :], ALPHA, ALU.mult, [('XH', c, t_) for t_ in range(NTT)], [('ACC', c, t_) for t_ in range(NTT)])
                      stt(ACC[:, c, :], XL[:, c, :], ALPHA, ACC[:, c, :], ALU.mult, ALU.add, [('XL', c, t_) for t_ in range(NTT)] + [('ACC', c, t_) for t_ in range(NTT)],
                          [('ACC', c, t_) for t_ in range(NTT)])
                  P.fence()
                  o3 = OFF_XL
                  def T3(shape, dtp):
                      nonlocal o3
                      nb = int(np.prod(shape[1:])) * (2 if dtp == BF16 else 4)
                      v = A(o3, shape, dtp); o3 += (nb + 3) // 4 * 4
                      return v
                  GT = T3([128, S], F32)
                  HT = [T3([128, 4, 512], BF16) for _ in range(2)]
                  SA = [T3([128, 512], BF16) for _ in range(2)]
                  TB = [T3([128, 512], BF16) for _ in range(2)]
                  GSB = T3([128, 512], F32)
                  RE = T3([128, 512], F32)
                  L_ = T3([128, NT, 16], F32); LM = T3([128, NT, 16], F32); IS1 = T3([128, NT, 16], F32); IS2 = T3([128, NT, 16], F32)
                  GG = T3([128, NT, 16], F32)
                  GMX = T3([128, NT, 4], F32); GMK = T3([128, NT, 4], F32); GT1 = T3([128, NT, 4], F32)
                  BMX = T3([128, NT], F32); M2 = T3([128, NT], F32); G1 = T3([128, NT], F32); G2 = T3([128, NT], F32)
                  assert o3 <= OFF_XL + SZA, (o3 - OFF_XL, SZA)
                  for i in range(NT):
                      for k in range(8):
                          mm(PS[6][:, i * 16:(i + 1) * 16], ACC[:, k, i * 128:(i + 1) * 128], RW[:, k, :], k == 0, k == 7, [('ACC', k, i // 4), 'rw'], [psk(6)])
                  stt(L_, PS[6][:, 0:NT * 16].rearrange("p (i e) -> p i e", e=16), 1.0 / ALPHA, RBT.unsqueeze(1).broadcast_to([128, NT, 16]), ALU.mult, ALU.add,
                      [psk(6), 'rbt'], ['L'])
                  red(GMX.rearrange("p i g -> p (i g)"), L_.rearrange("p i (g e) -> p (i g) e", g=4), ALU.max, ['L'], ['gmx'])
                  red(BMX, GMX, ALU.max, ['gmx'], ['bmx'])
                  tt(GMK, GMX, BMX.unsqueeze(2).broadcast_to([128, NT, 4]), ALU.is_equal, ['gmx', 'bmx'], ['gmk'])
                  ts(GT1, GMK, BIG, ALU.mult, ['gmk'], ['gt1'], s2=-BIG, op1=ALU.add)
                  L4 = lambda t_: t_.rearrange("p i (g e) -> p (i g) e", g=4)
                  G4 = lambda t_: t_.rearrange("p i g -> p (i g)").unsqueeze(2).broadcast_to([128, NT * 4, 4])
                  tt(L4(LM), L4(L_), G4(GMK), ALU.mult, ['L', 'gmk'], ['lm'])
                  tt(L4(LM), L4(LM), G4(GT1), ALU.add, ['lm', 'gt1'], ['lm'])
                  tt(IS1, LM, BMX.unsqueeze(2).broadcast_to([128, NT, 16]), ALU.is_equal, ['lm', 'bmx'], ['is1'])
                  stt(LM, IS1, -BIG, LM, ALU.mult, ALU.add, ['is1', 'lm'], ['lm'])
                  red(M2, LM, ALU.max, ['lm'], ['m2'])
                  tt(IS2, LM, M2.unsqueeze(2).broadcast_to([128, NT, 16]), ALU.is_equal, ['lm', 'm2'], ['is2'])
                  tt(G1, BMX, M2, ALU.subtract, ['bmx', 'm2'], ['g1'])
                  act(G1, G1, AF.Sigmoid, ['g1'], ['g1'])
                  ts(G2, G1, -1.0, ALU.mult, ['g1'], ['g2'], s2=1.0, op1=ALU.add)
                  tt(IS1, IS1, G1.unsqueeze(2).broadcast_to([128, NT, 16]), ALU.mult, ['is1', 'g1'], ['is1'])
                  tt(IS2, IS2, G2.unsqueeze(2).broadcast_to([128, NT, 16]), ALU.mult, ['is2', 'g2'], ['is2'])
                  tt(GG, IS1, IS2, ALU.add, ['is1', 'is2'], ['gg'])
                  for g in range(NTT):
                      for q in range(4):
                          i = g * 4 + q
                          tr(PS[6][0:16, q * 128:(q + 1) * 128], GG[:, i, :], IDF, ['gg', 'cst'], [psk(6)])
                      act(GT[0:16, g * 512:(g + 1) * 512], PS[6][0:16, :], AF.Copy, [psk(6)], ['gt'])
                  its = [(e, half, tti) for e in range(NE) for half in range(2) for tti in range(NTT)]
                  slabs_ = {}

                  def get_slabs(e, half):
                      if (e, half) not in slabs_:
                          sa_ = slab([(w1_d[l, e][:, half * 512:(half + 1) * 512], 0, 8, 0, 512)])
                          sb_ = slab([(w3_d[l, e][:, half * 512:(half + 1) * 512], 0, 8, 0, 512)])
                          sc_ = slab([(w2_d[l, e][half * 512:(half + 1) * 512, :], 0, 4, 0, 1024)], four=True)
                          slabs_[(e, half)] = (sa_, sb_, sc_)
                      return slabs_[(e, half)]

                  def stage_ab(n):
                      e, half, tti = its[n]
                      sa_, sb_, sc_ = get_slabs(e, half)
                      sl = slice(tti * 512, (tti + 1) * 512)
                      hb = n % 2
                      ts(RE[0:16, :], GT[0:16, sl], IDF[0:16, e:e + 1], ALU.mult, ['gt', 'cst'], ['re'])
                      mm(PS[6][:, :], ONES[0:16, :], RE[0:16, :], True, True, ['cst', 're'], [psk(6)])
                      act(GSB, PS[6][:, :], AF.Copy, [psk(6)], ['gsb'])
                      for hc in range(4):
                          fs = slice(hc * 128, (hc + 1) * 128)
                          b2 = hc % 2
                          for k in range(8):
                              mm(PS[b2][:, :], RING[sa_][:, k, fs], XH[:, k, sl], k == 0, k == 7, ringkeys(sa_) + [('XH', k, tti)], [psk(b2)])
                          act(SA[b2], PS[b2][:, :], AF.Silu, [psk(b2)], [('sa', b2)])
                          for k in range(8):
                              mm(PS[2 + b2][:, :], RING[sb_][:, k, fs], XH[:, k, sl], k == 0, k == 7, ringkeys(sb_) + [('XH', k, tti)], [psk(2 + b2)])
                          tt(TB[b2], PS[2 + b2][:, :], GSB, ALU.mult, [psk(2 + b2), 'gsb'], [('tb', b2)])
                          tt(HT[hb][:, hc, :], SA[b2], TB[b2], ALU.mult, [('sa', b2), ('tb', b2)], [('ht', hb, hc)])

                  def stage_w2(n):
                      e, half, tti = its[n]
                      sa_, sb_, sc_ = get_slabs(e, half)
                      sl = slice(tti * 512, (tti + 1) * 512)
                      hb = n % 2
                      for fc in range(8):
                          pb = 4 + (fc % 2)
                          for hc in range(4):
                              mm(PS[pb][:, :], RING4[sc_][:, hc, fc * 128:(fc + 1) * 128], HT[hb][:, hc, :], hc == 0, hc == 3, ringkeys(sc_) + [('ht', hb, hc)], [psk(pb)])
                          tt(ACC[:, fc, sl], PS[pb][:, :], ACC[:, fc, sl], ALU.add, [psk(pb), ('ACC', fc, tti)], [('ACC', fc, tti)])

                  stage_ab(0)
                  for n in range(len(its)):
                      if n + 1 < len(its):
                          stage_ab(n + 1)
                      stage_w2(n)
                  P.fence()
                  for tti in range(NTT):
                      sl = slice(tti * 512, (tti + 1) * 512)
                      ln_tile(lambda c: ACC[:, c, sl], lambda c: [('ACC', c, tti)], tti, lambda c: PF[:, 52 + c:53 + c], lambda c: PF[:, 60 + c:61 + c], 'pf', last, seq)
                  if seq == 0 and not last:
                      dump_fm('ln2', lambda c: XH[:, c, :], [k for t_ in range(NTT) for k in xkeys('XH', t_)])


        except _Stop:
            pass
        P.finalize()
        block.tensor(P.body('pe', engsem, dmasem))
        block.scalar(P.body('act', engsem, dmasem))
        block.vector(P.body('dve', engsem, dmasem))
        block.gpsimd(P.body('pool', engsem, dmasem))
        block.sync(P.body('sp', engsem, dmasem))
    return nc, P


def prep_inputs(inp):
    f = lambda a: np.ascontiguousarray(np.asarray(a, dtype=np.float32))
    b_in = f(inp['b_in'])
    L = b_in.shape[0]
    fm = lambda v: v.reshape(-1, 128).T
    pf = np.zeros((L, 128, NF), np.float32)
    pr = np.zeros((L, 1, NR), np.float32)
    for l in range(L):
        pf[l, :, 0:8] = fm(b_in[l, 0:1024])
        pf[l, :, 8:12] = fm(b_in[l, 1024:1536])
        pf[l, :, 12:28] = fm(b_in[l, 3592:5640])
        pf[l, :, 28:36] = fm(f(inp['b_mix_out'])[l])
        pf[l, :, 36:44] = fm(f(inp['ln1_g'])[l])
        pf[l, :, 44:52] = fm(f(inp['ln1_b'])[l])
        pf[l, :, 52:60] = fm(f(inp['ln2_g'])[l])
        pf[l, :, 60:68] = fm(f(inp['ln2_b'])[l])
        pf[l, 0:64, 68:76] = b_in[l, 1024:1536].reshape(8, 64).T
        pr[l, 0, 0:8] = b_in[l, 1536:1544]
        for j in range(4):
            for s in range(4):
                pr[l, 0, 8 + j * 512 + s * 128:8 + j * 512 + (s + 1) * 128] = b_in[l, 1544 + s * 512 + j * 128:1544 + s * 512 + (j + 1) * 128]
        pr[l, 0, 2056:2568] = f(inp['hgrn_norm_g'])[l]
    pfin = np.concatenate([fm(f(inp['ln_in_g'])), fm(f(inp['ln_in_b']))], axis=1)
    rw = f(inp['router_w']).reshape(8, 128, 16).transpose(1, 0, 2).reshape(128, 128)
    shared = {
        'w_in': f(inp['w_in']), 'w_fb': f(inp['w_fox_branch']), 'w_hb': f(inp['w_hgrn_branch']), 'w_mix': f(inp['w_mix_out']),
        'w1': f(inp['expert_w1']), 'w3': f(inp['expert_w3']), 'w2': f(inp['expert_w2']),
        'pf': pf, 'pfin': np.ascontiguousarray(pfin), 'pr': pr, 'lbl': f(inp['hgrn_lb_logits']).reshape(1, -1),
        'rw': np.ascontiguousarray(rw), 'rb': f(inp['router_b']).reshape(1, 16), 'cst': make_consts(),
    }
    return shared


_CACHE = {}


def kernel(**inputs):
    x = np.ascontiguousarray(np.asarray(inputs['x'], dtype=np.float32))
    B, S, _ = x.shape
    ncores = 8
    nseq = B // ncores
    depth = np.asarray(inputs['w_in']).shape[0]
    key = (nseq, S, depth)
    if key not in _CACHE:
        _CACHE[key] = build(nseq, S, list(range(depth)))[0]
    nc = _CACHE[key]
    shared = prep_inputs(inputs)
    in_maps = []
    for c in range(ncores):
        m = dict(shared)
        m['x'] = np.ascontiguousarray(x[c * nseq:(c + 1) * nseq])
        in_maps.append(m)
    res = run_bass_kernel_spmd(nc, in_maps, core_ids=list(range(ncores)))
    return np.concatenate([r['out'] for r in res.results], axis=0).astype(np.float32)
```
